# Optimizing a Trainium2 kernel written in Bass

```python
import jax, jax.numpy as jnp
from jax import lax
import numpy as np

D_MODEL = 1024
BATCH = 2
SEQ = 8192
DEPTH = 2

HEAD_DIM = 64
N_HEADS_MOBA = D_MODEL // 128
N_HEADS_SB = D_MODEL // 128
WIDTH_MOBA = N_HEADS_MOBA * HEAD_DIM
WIDTH_SB = N_HEADS_SB * HEAD_DIM
WIDTH_CONV = D_MODEL // 2
CONV_K = 3
MOBA_BLOCK = 256
MOBA_TOPK = 3
Q_BLOCK = 128
D_FF = -(-8 * D_MODEL // (3 * 256)) * 256
RMS_EPS = 1e-6
IN_SPLITS = (WIDTH_MOBA, WIDTH_MOBA, WIDTH_MOBA, WIDTH_SB, WIDTH_SB, WIDTH_SB,
             WIDTH_CONV, WIDTH_CONV, WIDTH_CONV, 3 * D_MODEL)
IN_WIDTH = sum(IN_SPLITS)

kernel_name = "hybrid_moba_stickbreak_shortconv_block"


def rms_norm(x, g):
    xf = x.astype(jnp.float32)
    y = xf * lax.rsqrt(jnp.mean(xf * xf, axis=-1, keepdims=True) + RMS_EPS)
    return (y * g.astype(jnp.float32)).astype(x.dtype)


def alibi_slopes(n_heads):
    return jnp.exp2(-8.0 * (jnp.arange(n_heads, dtype=jnp.float32) + 1.0) / n_heads)


def moba_attention(q, k, v):
    B, S, H, d = q.shape
    nq = S // Q_BLOCK
    nb = -(-S // MOBA_BLOCK)
    s_pad = nb * MOBA_BLOCK
    topk = min(MOBA_TOPK, nb)
    scale = d ** -0.5
    slopes = alibi_slopes(H)
    pad = ((0, 0), (0, 0), (0, s_pad - S), (0, 0))
    kt = jnp.pad(k.transpose(0, 2, 1, 3), pad)
    vt = jnp.pad(v.transpose(0, 2, 1, 3), pad)
    kb = kt.reshape(B, H, nb, MOBA_BLOCK, d)
    vb = vt.reshape(B, H, nb, MOBA_BLOCK, d)
    kmean = jnp.mean(kb.astype(jnp.float32), axis=3).astype(k.dtype)
    qc_all = q.reshape(B, nq, Q_BLOCK, H, d).transpose(1, 0, 3, 2, 4)
    bi = jnp.arange(B)[:, None, None, None]
    hi = jnp.arange(H)[None, :, None, None]
    offs = jnp.arange(MOBA_BLOCK)
    blk_ids = jnp.arange(nb)

    def chunk(args):
        c, qc = args
        t = c * Q_BLOCK + jnp.arange(Q_BLOCK)
        blk = (c * Q_BLOCK) // MOBA_BLOCK
        gate = jnp.einsum('bhqd,bhnd->bhqn', qc, kmean).astype(jnp.float32)
        gate = jnp.where(blk_ids < blk, gate, -jnp.inf)
        _, idx = lax.top_k(gate, topk)
        kg = kb[bi, hi, idx]
        vg = vb[bi, hi, idx]
        s_sel = jnp.einsum('bhqd,bhqrld->bhqrl', qc, kg).astype(jnp.float32) * scale
        pos_sel = idx[..., None] * MOBA_BLOCK + offs
        dist_sel = (t[None, None, :, None, None] - pos_sel).astype(jnp.float32)
        s_sel = s_sel - slopes[None, :, None, None, None] * dist_sel
        valid = jnp.arange(topk) < blk
        s_sel = jnp.where(valid[:, None], s_sel, -jnp.inf)
        ko = lax.dynamic_index_in_dim(kb, blk, axis=2, keepdims=False)
        vo = lax.dynamic_index_in_dim(vb, blk, axis=2, keepdims=False)
        s_own = jnp.einsum('bhqd,bhld->bhql', qc, ko).astype(jnp.float32) * scale
        dist_own = t[:, None] - (blk * MOBA_BLOCK + offs)[None, :]
        s_own = s_own - slopes[None, :, None, None] * dist_own.astype(jnp.float32)
        s_own = jnp.where(dist_own >= 0, s_own, -jnp.inf)
        n_sel = topk * MOBA_BLOCK
        scores = jnp.concatenate([s_sel.reshape(B, H, Q_BLOCK, n_sel), s_own], axis=-1)
        p = jax.nn.softmax(scores, axis=-1).astype(v.dtype)
        p_sel = p[..., :n_sel].reshape(B, H, Q_BLOCK, topk, MOBA_BLOCK)
        p_own = p[..., n_sel:]
        return (jnp.einsum('bhqrl,bhqrld->bhqd', p_sel, vg)
                + jnp.einsum('bhql,bhld->bhqd', p_own, vo))

    out = lax.map(chunk, (jnp.arange(nq), qc_all))
    return out.transpose(1, 0, 3, 2, 4).reshape(B, S, H * d)


def stick_breaking_attention(q, k, v):
    B, S, H, d = q.shape
    nq = S // Q_BLOCK
    scale = d ** -0.5
    kt = k.transpose(0, 2, 1, 3)
    vt = v.transpose(0, 2, 1, 3)
    qc_all = q.reshape(B, nq, Q_BLOCK, H, d).transpose(1, 0, 3, 2, 4)
    s_pos = jnp.arange(S)

    def chunk(args):
        c, qc = args
        t = c * Q_BLOCK + jnp.arange(Q_BLOCK)
        z = jnp.einsum('bhqd,bhsd->bhqs', qc, kt).astype(jnp.float32) * scale
        strict = s_pos[None, :] < t[:, None]
        log_not = jnp.where(strict, jax.nn.log_sigmoid(-z), 0.0)
        after = lax.cumsum(log_not, axis=3, reverse=True) - log_not
        w = jnp.where(strict, jnp.exp(jax.nn.log_sigmoid(z) + after), 0.0)
        return jnp.einsum('bhqs,bhsd->bhqd', w.astype(v.dtype), vt)

    out = lax.map(chunk, (jnp.arange(nq), qc_all))
    return out.transpose(1, 0, 3, 2, 4).reshape(B, S, H * d)


def short_conv_mixer(xc, bg, cg, conv_w):
    u = cg * xc
    S = u.shape[1]
    up = jnp.pad(u, ((0, 0), (CONV_K - 1, 0), (0, 0)))
    y = up[:, 0:S] * conv_w[0]
    for j in range(1, CONV_K):
        y = y + up[:, j:j + S] * conv_w[j]
    return bg * y


def hybrid_layer(x, g_mix, w_in, b_gate, conv_w, w_proj_moba, w_proj_sb, w_proj_conv,
                 w_out, g_ffn, w_ffn_gate, w_ffn_up, w_ffn_down):
    B, S, _ = x.shape
    h = rms_norm(x, g_mix)
    proj = jnp.einsum('bsd,de->bse', h, w_in)
    offsets = np.cumsum(IN_SPLITS)[:-1].tolist()
    qa, ka, va, qs, ks, vs, xc, bc, cc, gates = jnp.split(proj, offsets, axis=-1)
    o_a = moba_attention(qa.reshape(B, S, N_HEADS_MOBA, HEAD_DIM),
                         ka.reshape(B, S, N_HEADS_MOBA, HEAD_DIM),
                         va.reshape(B, S, N_HEADS_MOBA, HEAD_DIM))
    o_b = stick_breaking_attention(qs.reshape(B, S, N_HEADS_SB, HEAD_DIM),
                                   ks.reshape(B, S, N_HEADS_SB, HEAD_DIM),
                                   vs.reshape(B, S, N_HEADS_SB, HEAD_DIM))
    o_c = short_conv_mixer(xc, bc, cc, conv_w)
    g = jax.nn.sigmoid((gates + b_gate).astype(jnp.float32)).astype(x.dtype)
    g_a, g_b, g_c = jnp.split(g, 3, axis=-1)
    merged = (g_a * (o_a @ w_proj_moba) + g_b * (o_b @ w_proj_sb)
              + g_c * (o_c @ w_proj_conv))
    x = x + merged @ w_out
    h2 = rms_norm(x, g_ffn)
    x = x + (jax.nn.silu(h2 @ w_ffn_gate) * (h2 @ w_ffn_up)) @ w_ffn_down
    return x


def setup_inputs(seed: int = 0) -> dict:
    key = jax.random.key(seed)
    ks = jax.random.split(key, 16)

    def nrm(k, shape, fan_in):
        return jax.random.normal(k, shape, jnp.float32) * (fan_in ** -0.5)

    def gain(k, shape):
        return 1.0 + 0.02 * jax.random.normal(k, shape, jnp.float32)

    return {
        "x": jax.random.normal(ks[0], (BATCH, SEQ, D_MODEL), jnp.float32),
        "norm_mix_g": gain(ks[1], (DEPTH, D_MODEL)),
        "w_in": nrm(ks[2], (DEPTH, D_MODEL, IN_WIDTH), D_MODEL),
        "b_gate": 0.02 * jax.random.normal(ks[3], (DEPTH, 3 * D_MODEL), jnp.float32),
        "conv_w": nrm(ks[4], (DEPTH, CONV_K, WIDTH_CONV), CONV_K),
        "w_proj_moba": nrm(ks[5], (DEPTH, WIDTH_MOBA, D_MODEL), WIDTH_MOBA),
        "w_proj_sb": nrm(ks[6], (DEPTH, WIDTH_SB, D_MODEL), WIDTH_SB),
        "w_proj_conv": nrm(ks[7], (DEPTH, WIDTH_CONV, D_MODEL), WIDTH_CONV),
        "w_out": nrm(ks[8], (DEPTH, D_MODEL, D_MODEL), D_MODEL),
        "norm_ffn_g": gain(ks[9], (DEPTH, D_MODEL)),
        "w_ffn_gate": nrm(ks[10], (DEPTH, D_MODEL, D_FF), D_MODEL),
        "w_ffn_up": nrm(ks[11], (DEPTH, D_MODEL, D_FF), D_MODEL),
        "w_ffn_down": nrm(ks[12], (DEPTH, D_FF, D_MODEL), D_FF),
        "norm_final_g": gain(ks[13], (D_MODEL,)),
    }


def reference(x, norm_mix_g, w_in, b_gate, conv_w, w_proj_moba, w_proj_sb, w_proj_conv,
              w_out, norm_ffn_g, w_ffn_gate, w_ffn_up, w_ffn_down, norm_final_g):
    for l in range(DEPTH):
        x = hybrid_layer(x, norm_mix_g[l], w_in[l], b_gate[l], conv_w[l],
                         w_proj_moba[l], w_proj_sb[l], w_proj_conv[l], w_out[l],
                         norm_ffn_g[l], w_ffn_gate[l], w_ffn_up[l], w_ffn_down[l])
    return rms_norm(x, norm_final_g)
```

```python
import numpy as np
import ml_dtypes
from contextlib import ExitStack
import concourse.bass as bass
import concourse.mybir as mybir
from concourse.bass_utils import run_bass_kernel_spmd

F32 = mybir.dt.float32
BF16 = mybir.dt.bfloat16
AF = mybir.ActivationFunctionType
ALU = mybir.AluOpType
AX = mybir.AxisListType

NCORES = 8
D = 1024
B = 2
S = 8192
NTOK = B * S
TSH = NTOK // NCORES
DEPTH = 2
DFF = 2816
NFC = DFF // 128
INW = 7680
EPS = 1e-6
SCALE = 0.125
BIG = 16384.0
TT = 512
NSUB = TSH // TT
NQT = S // TT
NKC = S // 128
NBLK = S // 256
DBG_STOP = 0


class Res:
    __slots__ = ("w", "r", "excl")

    def __init__(self, excl=False):
        self.w = None
        self.r = {}
        self.excl = excl


def mkres(n, excl=False):
    return [Res(excl) for _ in range(n)]


class Sched:
    ENGS = ("pe", "act", "dve", "pool", "sp")
    QSLOTS = {"sp": list(range(0, 8)), "pool": list(range(8, 14)), "act": list(range(14, 16))}

    def __init__(self):
        self.q = {e: [] for e in self.ENGS}
        self.n = {e: 0 for e in self.ENGS}
        self.flag = {e: set() for e in self.ENGS}
        self.waited = {e: {} for e in self.ENGS}
        self.slot_n = [0] * 16
        self.slot_rr = {k: 0 for k in self.QSLOTS}

    def _need(self, eng, reads, writes):
        need = {}
        for r in reads:
            if r.w is not None:
                k, v = r.w
                if k == eng and eng == "pe":
                    continue
                if v > need.get(k, 0):
                    need[k] = v
            if r.excl:
                for k, v in r.r.items():
                    if k != eng and v > need.get(k, 0):
                        need[k] = v
        for w in writes:
            if w.w is not None:
                k, v = w.w
                if k != eng and v > need.get(k, 0):
                    need[k] = v
            for k, v in w.r.items():
                if k != eng and v > need.get(k, 0):
                    need[k] = v
        return need

    def _waits(self, eng, need):
        wd = self.waited[eng]
        for k, v in need.items():
            if wd.get(k, 0) >= v:
                continue
            wd[k] = v
            if k[0] != "q":
                self.flag[k].add(v)
            self.q[eng].append(("wait", k, v))

    def op(self, eng, fn, reads=(), writes=()):
        self._waits(eng, self._need(eng, reads, writes))
        self.n[eng] += 1
        idx = self.n[eng]
        self.q[eng].append(("op", fn, idx))
        for r in reads:
            r.r[eng] = idx
        for w in writes:
            w.w = (eng, idx)
            w.r = {}
        return idx

    def dma(self, queue, out, in_, reads=(), writes=()):
        sl = self.QSLOTS[queue]
        slot = sl[self.slot_rr[queue] % len(sl)]
        self.slot_rr[queue] += 1
        key = "q%d" % slot
        need = self._need(None, reads, writes)
        if self.slot_n[slot] > 0:
            need[key] = max(need.get(key, 0), self.slot_n[slot])
        self._waits(queue, need)
        self.slot_n[slot] += 1
        v = self.slot_n[slot]
        self.q[queue].append(("dma", out, in_, slot))
        for r in reads:
            r.r[key] = v
        for w in writes:
            w.w = (key, v)
            w.r = {}

    def finish(self, queue):
        need = {}
        for s in range(16):
            if self.slot_n[s] > 0:
                need["q%d" % s] = self.slot_n[s]
        self._waits(queue, need)

    def emit(self, nc, stack):
        sems = {}
        for k in ("pe", "act", "dve", "pool"):
            sems[k] = stack.enter_context(nc.semaphore("s_" + k))
        for s in range(16):
            sems["q%d" % s] = stack.enter_context(nc.semaphore("s_q%d" % s))
        rank = {}
        for k in self.ENGS:
            fl = sorted(self.flag[k])
            rank[k] = {idx: i + 1 for i, idx in enumerate(fl)}
        block = stack.enter_context(nc.Block())
        q = self.q

        def run(ek, e):
            rk = rank[ek]
            for item in q[ek]:
                t = item[0]
                if t == "wait":
                    _, k, v = item
                    val = 16 * v if k[0] == "q" else rank[k][v]
                    e.wait_ge(sems[k], val)
                elif t == "op":
                    inst = item[1](e)
                    if item[2] in rk:
                        inst.then_inc(sems[ek], 1)
                else:
                    _, out, in_, slot = item
                    e.dma_start(out=out, in_=in_).then_inc(sems["q%d" % slot], 16)

        @block.tensor
        def _(e):
            run("pe", e)

        @block.scalar
        def _(e):
            run("act", e)

        @block.vector
        def _(e):
            run("dve", e)

        @block.gpsimd
        def _(e):
            run("pool", e)

        @block.sync
        def _(e):
            run("sp", e)


class Ctx:
    def __init__(self, nc, stack):
        self.nc = nc
        self.stack = stack
        self.S = Sched()
        self._n = 0

    def sb(self, shape, dt, name=None):
        self._n += 1
        return self.stack.enter_context(self.nc.sbuf_tensor("%s_%d" % (name or "sb", self._n), list(shape), dt))

    def ps(self, name=None):
        self._n += 1
        return self.stack.enter_context(self.nc.psum_tensor("%s_%d" % (name or "ps", self._n), [128, 512], F32))


def mm(S, out, lhsT, rhs, start, stop, reads, writes):
    S.op("pe", lambda e: e.matmul(out, lhsT, rhs, start=start, stop=stop, skip_group_check=True), reads, writes)


CB_ONES = 0
CB_IDENT = 128
CB_NTRI = 256
CB_NLOW = 384
CB_MLE = 512
CB_MLT = 512 + 2048
CB_W = 512 + 4096


def build_cb():
    cb = np.zeros((128, CB_W), np.float32)
    cb[:, CB_ONES:CB_ONES + 128] = 1.0
    cb[:, CB_IDENT:CB_IDENT + 128] = np.eye(128)
    jj = np.arange(128)[:, None]
    ss = np.arange(128)[None, :]
    cb[:, CB_NTRI:CB_NTRI + 128] = -1.0 * (jj >= ss)
    cb[:, CB_NLOW:CB_NLOW + 128] = -1.0 * (jj < ss)
    t = np.arange(512)[None, :]
    for j in range(4):
        cb[:, CB_MLE + 512 * j:CB_MLE + 512 * (j + 1)] = (128 * j + jj <= t)
        cb[:, CB_MLT + 512 * j:CB_MLT + 512 * (j + 1)] = (128 * j + jj < t)
    return cb.astype(ml_dtypes.bfloat16)


def build_cfB(slope):
    cf = np.zeros((128, 128), np.float32)
    j = np.arange(64)
    m = np.where(j < 32, 0.0, np.where(j == 32, BIG, -BIG))
    ab = np.where(j <= 32, BIG - slope * 256.0 * (32 - j), 0.0)
    cf[:, 0:64] = m[None, :]
    cf[:, 64:128] = ab[None, :]
    return cf


def build_kq_const(slope):
    s = np.arange(S)
    kc = np.zeros((35, S), np.float32)
    kc[s // 256, s] = 1.0
    kc[32, :] = 1.0
    kc[33, :] = slope * (s % 256)
    kc[34, :] = 1.0
    qc = np.zeros((2, S), np.float32)
    qc[0, :] = 1.0
    qc[1, :] = -slope * (s % 256)
    return kc.astype(ml_dtypes.bfloat16), qc.astype(ml_dtypes.bfloat16)


def phase_B(cx, hT_all, wB, cb_d, cfB_d, kconst_d, qconst_d, o_loc):
    S_ = cx.S
    cb = cx.sb([128, CB_W], BF16, "cb")
    cf = cx.sb([128, 128], F32, "cfB")
    onesf = cx.sb([128, 64], F32, "onesf")
    wq = cx.sb([128, 8, 384], BF16, "wq")
    Qa = cx.sb([128, S], BF16, "Qa")
    Ka = cx.sb([128, S], BF16, "Ka")
    Va = cx.sb([128, NKC, 65], BF16, "Va")
    Qs = cx.sb([128, S], BF16, "Qs")
    Ks = cx.sb([128, S], BF16, "Ks")
    Vs = cx.sb([128, NKC, 64], BF16, "Vs")
    hbuf = [cx.sb([128, 8, TT], BF16, "hb") for _ in range(2)]
    kmean = cx.sb([128, 32], F32, "kmean")
    kmean_b = cx.sb([128, 32], BF16, "kmeanb")
    kmaxt = cx.sb([128, NQT], F32, "kmaxt")
    negc = cx.sb([128, 1], F32, "negc")
    sq = cx.sb([128, TT], BF16, "sq")
    gate = cx.sb([128, 32], F32, "gate")
    top8 = cx.sb([128, 8], F32, "top8")
    thr = cx.sb([128, 1], F32, "thr")
    sel = cx.sb([128, 32], F32, "sel")
    mpad = [cx.sb([128, 96], BF16, "mpad") for _ in range(2)]
    nqb = [cx.sb([128, TT], BF16, "nq") for _ in range(2)]
    Ebuf = [cx.sb([128, TT], F32, "E") for _ in range(2)]
    SPb = [cx.sb([128, TT], BF16, "SP") for _ in range(3)]
    Wb = [cx.sb([128, TT], BF16, "W") for _ in range(2)]
    Pb = [cx.sb([128, TT], BF16, "P") for _ in range(2)]
    rl = cx.sb([128, TT], F32, "rl")
    oaf = cx.sb([128, TT], F32, "oaf")
    oout = [cx.sb([128, TT], BF16, "oout") for _ in range(2)]
    pz = [cx.ps("pz") for _ in range(2)]
    psm = [cx.ps("psm") for _ in range(2)]
    pR = cx.ps("pR")
    pOs = cx.ps("pOs")
    pOa = cx.ps("pOa")
    pX = cx.ps("pX")

    r_cb, r_cf, r_onesf, r_wq = Res(), Res(), Res(), Res()
    r_Qa = mkres(NQT)
    r_Qm = mkres(NQT)
    r_Qst = mkres(NQT)
    r_Qc = Res()
    r_Ka = mkres(NQT)
    r_Kc = Res()
    r_Va = mkres(NQT)
    r_Vone = Res()
    r_Qs = mkres(NQT)
    r_Ks = mkres(NQT)
    r_Vs = mkres(NQT)
    r_hb = mkres(2)
    r_kmean, r_kmeanb, r_kmaxt, r_negc, r_sq = Res(), Res(), Res(), Res(), Res()
    r_gate, r_top8, r_thr, r_sel = Res(), Res(), Res(), Res()
    r_mpad = mkres(2)
    r_nq = mkres(2)
    r_E = mkres(2)
    r_SP = mkres(3)
    r_W = mkres(2)
    r_P = mkres(2)
    r_rl, r_oaf = Res(), Res()
    r_oout = mkres(2)
    r_pz = mkres(2, True)
    r_psm = mkres(2, True)
    r_pR, r_pOs, r_pOa, r_pX = Res(True), Res(True), Res(True), Res(True)

    S_.dma("sp", cb[:], cb_d, [], [r_cb])
    S_.dma("sp", cf[:], cfB_d, [], [r_cf])
    S_.dma("pool", wq[:], wB.rearrange("(k p) n -> p k n", p=128), [], [r_wq])
    S_.op("pool", lambda e: e.memset(onesf[:], 1.0), [], [r_onesf])
    S_.op("pool", lambda e: e.memset(kmean[:], 0.0), [], [r_kmean])
    S_.op("pool", lambda e: e.memset(Va[:, :, 64:65], 1.0), [], [r_Vone])
    for i in range(2):
        S_.op("pool", lambda e, i=i: e.memset(mpad[i][:], 0.0), [], [r_mpad[i]])

    if DBG_STOP == 10:
        S_.dma("sp", o_loc[:, 0:384], wq[:, 0, :], [r_wq, r_cb, r_cf, r_onesf, r_Vone, r_kmean] + r_mpad, [])
        S_.finish("sp")
        return
    ones_bf = cb[:, CB_ONES:CB_ONES + 128]
    ident = cb[:, CB_IDENT:CB_IDENT + 128]
    ntri = cb[:, CB_NTRI:CB_NTRI + 128]
    nlow = cb[:, CB_NLOW:CB_NLOW + 128]

    for b in range(B):
        if b == 0:
            S_.dma("sp", Ka[64:99, :], kconst_d, [], [r_Kc])
            S_.dma("sp", Qa[97:99, :], qconst_d, [], [r_Qc])
        for j in range(NQT):
            hb = hbuf[j % 2]
            rh = r_hb[j % 2]
            rank, tl = divmod(b * S + j * TT, TSH)
            S_.dma("sp", hb[:], hT_all[rank, :, tl:tl + TT].rearrange("(k p) t -> p k t", p=128), [], [rh])
            tsl = slice(j * TT, (j + 1) * TT)
            tgt = [(pz[0], r_pz[0], 0), (pz[1], r_pz[1], 64), (psm[0], r_psm[0], 128), (psm[1], r_psm[1], 192)]
            for (pt, rp, c0) in tgt:
                for k in range(8):
                    mm(S_, pt[0:64, :], wq[:, k, c0:c0 + 64], hb[:, k, :], k == 0, k == 7, [r_wq, rh], [rp])
            for c in range(4):
                for k in range(8):
                    mm(S_, pX[:, c * 128:(c + 1) * 128], hb[:, k, c * 128:(c + 1) * 128], wq[:, k, 256:384],
                       k == 0, k == 7, [r_wq, rh], [r_pX])
            if DBG_STOP == 11:
                S_.op("dve", lambda e: e.tensor_copy(out=Ka[0:64, 0:512], in_=pz[1][0:64, :]), [r_pz[1]], [r_Ka[j]])
                S_.op("dve", lambda e: e.tensor_copy(out=Qa[0:64, 0:512], in_=pX[0:64, :]), [r_pX, r_pz[0], r_psm[0], r_psm[1]], [r_Qa[j]])
                S_.dma("sp", o_loc[0:64, 0:512], Ka[0:64, 0:512], [r_Ka[j], r_Qa[j]], [])
                S_.finish("sp")
                return
            S_.op("act", lambda e, tsl=tsl: e.mul(out=Qa[0:64, tsl], in_=pz[0][0:64, :], mul=SCALE),
                  [r_pz[0]], [r_Qa[j]])
            S_.op("dve", lambda e, tsl=tsl: e.tensor_copy(out=Ka[0:64, tsl], in_=pz[1][0:64, :]), [r_pz[1]], [r_Ka[j]])
            S_.op("dve", lambda e, j=j: e.tensor_reduce(out=kmean[0:64, 2 * j:2 * j + 2],
                                                        in_=pz[1][0:64, :].rearrange("p (a b) -> p a b", b=256),
                                                        axis=AX.X, op=ALU.add), [r_pz[1]], [r_kmean])
            S_.op("act", lambda e, tsl=tsl: e.mul(out=Qs[0:64, tsl], in_=psm[0][0:64, :], mul=SCALE),
                  [r_psm[0]], [r_Qs[j]])
            S_.op("dve", lambda e, tsl=tsl: e.tensor_copy(out=Ks[0:64, tsl], in_=psm[1][0:64, :]), [r_psm[1]], [r_Ks[j]])
            pXv = pX[:].rearrange("p (c n) -> p c n", n=128)
            S_.op("act", lambda e, j=j, pXv=pXv: e.copy(out=Va[:, 4 * j:4 * j + 4, 0:64], in_=pXv[:, :, 0:64]),
                  [r_pX], [r_Va[j]])
            S_.op("dve", lambda e, j=j, pXv=pXv: e.tensor_copy(out=Vs[:, 4 * j:4 * j + 4, :], in_=pXv[:, :, 64:128]),
                  [r_pX], [r_Vs[j]])
            if DBG_STOP == 12:
                S_.dma("sp", o_loc[0:64, 0:512], Ka[0:64, 0:512], [r_Ka[j], r_Qa[j], r_Qs[j], r_Ks[j], r_Va[j], r_Vs[j], r_kmean], [])
                S_.finish("sp")
                return
            S_.op("pool", lambda e, tsl=tsl: e.tensor_tensor(out=sq[0:64, :], in0=Ka[0:64, tsl], in1=Ka[0:64, tsl], op=ALU.mult),
                  [r_Ka[j]], [r_sq])
            mm(S_, pR[0:97, :], ones_bf[0:64, 0:97], sq[0:64, :], True, True, [r_cb, r_sq], [r_pR])
            S_.op("dve", lambda e, j=j: e.tensor_reduce(out=kmaxt[0:97, j:j + 1], in_=pR[0:97, :], axis=AX.X, op=ALU.max),
                  [r_pR], [r_kmaxt])
        if DBG_STOP == 13:
            S_.dma("sp", o_loc[0:64, 0:512], Ka[0:64, 0:512], [r_Ka[0], r_kmaxt], [])
            S_.finish("sp")
            return
        S_.op("dve", lambda e: e.tensor_reduce(out=negc[0:97, :], in_=kmaxt[0:97, :], axis=AX.X, op=ALU.max), [r_kmaxt], [r_negc])
        S_.op("dve", lambda e: e.tensor_scalar(out=negc[0:97, :], in0=negc[0:97, :], scalar1=-0.5 * SCALE, scalar2=None, op0=ALU.mult),
              [r_negc], [r_negc])
        S_.op("dve", lambda e: e.tensor_copy(out=kmean_b[0:64, :], in_=kmean[0:64, :]), [r_kmean], [r_kmeanb])
        if DBG_STOP == 1:
            S_.dma("sp", o_loc[0:64, 0:S], Qa[0:64, :], r_Qa, [])
            S_.dma("sp", o_loc[64:128, 0:S], Ks[0:64, :], r_Ks, [])
            break
        for c in range(S // 128):
            blk = c // 2
            jt = c // 4
            csl = slice(c * 128, (c + 1) * 128)
            mp = mpad[c % 2]
            rmp = r_mpad[c % 2]
            mm(S_, pX[:, 0:32], Qa[0:64, csl], kmean_b[0:64, :], True, True, [r_Qa[jt], r_kmeanb], [r_pX])
            S_.op("dve", lambda e, blk=blk: e.tensor_tensor(out=gate[:], in0=pX[:, 0:32], in1=cf[:, 32 - blk:64 - blk], op=ALU.add),
                  [r_pX, r_cf], [r_gate])
            S_.op("dve", lambda e: e.max(out=top8[:], in_=gate[:]), [r_gate], [r_top8])
            S_.op("dve", lambda e: e.tensor_scalar(out=thr[:], in0=top8[:, 3:4], scalar1=-BIG / 2, scalar2=None, op0=ALU.max),
                  [r_top8], [r_thr])
            S_.op("dve", lambda e: e.tensor_scalar(out=sel[:], in0=gate[:], scalar1=thr[:, 0:1], scalar2=None, op0=ALU.is_ge),
                  [r_gate, r_thr], [r_sel])
            S_.op("dve", lambda e, blk=blk: e.tensor_tensor(out=sel[:], in0=sel[:], in1=cf[:, 64 + 32 - blk:64 + 64 - blk], op=ALU.mult),
                  [r_sel, r_cf], [r_sel])
            S_.op("dve", lambda e, mp=mp: e.tensor_scalar(out=mp[:, 64:96], in0=sel[:], scalar1=-BIG, scalar2=None, op0=ALU.add),
                  [r_sel], [rmp])
            mm(S_, psm[c % 2][0:96, 0:128], mp[:, 0:96], ident, True, True, [rmp, r_cb], [r_psm[c % 2]])
            S_.op("act", lambda e, c=c, csl=csl: e.copy(out=Qa[64:96, csl], in_=psm[c % 2][64:96, 0:128]),
                  [r_psm[c % 2]], [r_Qm[jt]])
        if DBG_STOP == 2:
            S_.dma("sp", o_loc[0:99, 0:S], Qa[0:99, :], r_Qa + r_Qm + [r_Qc], [])
            break
        pairs = []
        for qt in range(NQT):
            n = 4 * qt + 4
            for i in range(n):
                pairs.append((qt, i, n))
        NP = len(pairs)

        def cols_of(kc, qt):
            j = kc - 4 * qt
            return (128 * j if j >= 0 else 0), j

        def sb_kc(p):
            qt, i, n = pairs[p]
            return n - 1 - i

        def mo_kc(p):
            qt, i, n = pairs[p]
            return i

        def tile_init(qt):
            tsl = slice(qt * TT, (qt + 1) * TT)
            S_.op("dve", lambda e: e.tensor_tensor(out=sq[0:64, :], in0=Qa[0:64, tsl], in1=Qa[0:64, tsl], op=ALU.mult),
                  [r_Qa[qt]], [r_sq])
            mm(S_, pX[0:97, :], ones_bf[0:64, 0:97], sq[0:64, :], True, True, [r_cb, r_sq], [r_pX])
            S_.op("dve", lambda e: e.tensor_scalar(out=Qa[96:97, tsl], in0=pX[96:97, :], scalar1=-0.5 / SCALE,
                                                   scalar2=negc[96:97, 0:1], op0=ALU.mult, op1=ALU.add),
                  [r_pX, r_negc], [r_Qst[qt]])
            nq = nqb[qt % 2]
            S_.op("pool", lambda e: e.tensor_scalar(out=nq[0:64, :], in0=Qs[0:64, tsl], scalar1=-1.0, scalar2=None, op0=ALU.mult),
                  [r_Qs[qt]], [r_nq[qt % 2]])

        def tile_fin_sb(qt):
            tsl = slice(b * S + qt * TT, b * S + (qt + 1) * TT)
            ob = oout[1]
            S_.op("dve", lambda e: e.tensor_copy(out=ob[0:64, :], in_=pOs[0:64, :]), [r_pOs], [r_oout[1]])
            S_.dma("sp", o_loc[64:128, tsl], ob[0:64, :], [r_oout[1]], [])

        def tile_fin_mo(qt):
            tsl = slice(b * S + qt * TT, b * S + (qt + 1) * TT)
            ob = oout[0]
            S_.op("dve", lambda e: e.reciprocal(out=rl[64:65, :], in_=pOa[64:65, :]), [r_pOa], [r_rl])
            S_.op("act", lambda e: e.copy(out=oaf[0:64, :], in_=pOa[0:64, :]), [r_pOa], [r_oaf])
            mm(S_, pX[0:64, :], onesf[64:65, 0:64], rl[64:65, :], True, True, [r_onesf, r_rl], [r_pX])
            S_.op("dve", lambda e: e.tensor_tensor(out=ob[0:64, :], in0=oaf[0:64, :], in1=pX[0:64, :], op=ALU.mult),
                  [r_oaf, r_pX], [r_oout[0]])
            S_.dma("sp", o_loc[0:64, tsl], ob[0:64, :], [r_oout[0]], [])

        def f_Z(p):
            qt, i, n = pairs[p]
            if i == 0:
                tile_init(qt)
            kc = sb_kc(p)
            c0, j = cols_of(kc, qt)
            tq = slice(qt * TT + c0, (qt + 1) * TT)
            mm(S_, pz[p % 2][:, c0:TT], Ks[0:64, kc * 128:(kc + 1) * 128], Qs[0:64, tq], True, True,
               [r_Ks[kc // 4], r_Qs[qt]], [r_pz[p % 2]])

        def f_ESP(p):
            qt, i, n = pairs[p]
            kc = sb_kc(p)
            c0, j = cols_of(kc, qt)
            E = Ebuf[p % 2]
            SP = SPb[p % 3]
            S_.op("act", lambda e: e.activation(out=E[:, c0:TT], in_=pz[p % 2][:, c0:TT], func=AF.Exp), [r_pz[p % 2]], [r_E[p % 2]])
            S_.op("act", lambda e: e.activation(out=SP[:, c0:TT], in_=E[:, c0:TT], func=AF.Ln, bias=1.0), [r_E[p % 2]], [r_SP[p % 3]])
            if j >= 0:
                mk = cb[:, CB_MLT + 512 * j + c0:CB_MLT + 512 * (j + 1)]
                S_.op("pool", lambda e: e.tensor_tensor(out=SP[:, c0:TT], in0=SP[:, c0:TT], in1=mk, op=ALU.mult),
                      [r_SP[p % 3], r_cb], [r_SP[p % 3]])

        def f_R(p):
            qt, i, n = pairs[p]
            kc = sb_kc(p)
            c0, j = cols_of(kc, qt)
            tq = slice(qt * TT + c0, (qt + 1) * TT)
            SP = SPb[p % 3]
            mm(S_, pR[:, c0:TT], Ks[0:64, kc * 128:(kc + 1) * 128], Qs[0:64, tq], i == 0, False,
               [r_Ks[kc // 4], r_Qs[qt]], [r_pR])
            mm(S_, pR[:, c0:TT], ntri, SP[:, c0:TT], False, True, [r_cb, r_SP[p % 3]], [r_pR])

        def f_W(p):
            qt, i, n = pairs[p]
            kc = sb_kc(p)
            c0, j = cols_of(kc, qt)
            W = Wb[p % 2]
            S_.op("act", lambda e: e.activation(out=W[:, c0:TT], in_=pR[:, c0:TT], func=AF.Exp), [r_pR], [r_W[p % 2]])
            if j >= 0:
                mk = cb[:, CB_MLT + 512 * j + c0:CB_MLT + 512 * (j + 1)]
                S_.op("pool", lambda e: e.tensor_tensor(out=W[:, c0:TT], in0=W[:, c0:TT], in1=mk, op=ALU.mult),
                      [r_W[p % 2], r_cb], [r_W[p % 2]])

        def f_corr(p):
            qt, i, n = pairs[p]
            kc = sb_kc(p)
            c0, j = cols_of(kc, qt)
            SP = SPb[p % 3]
            W = Wb[p % 2]
            if i < n - 1:
                mm(S_, pR[:, c0:TT], Ks[0:64, kc * 128:(kc + 1) * 128], nqb[qt % 2][0:64, c0:TT], False, False,
                   [r_Ks[kc // 4], r_nq[qt % 2]], [r_pR])
                mm(S_, pR[:, c0:TT], nlow, SP[:, c0:TT], False, True, [r_cb, r_SP[p % 3]], [r_pR])
            mm(S_, pOs[0:64, c0:TT], Vs[:, kc, :], W[:, c0:TT], i == 0, i == n - 1, [r_Vs[kc // 4], r_W[p % 2]], [r_pOs])
            if i == n - 1:
                tile_fin_sb(qt)

        def f_Sm(p):
            qt, i, n = pairs[p]
            kc = mo_kc(p)
            c0, j = cols_of(kc, qt)
            tq = slice(qt * TT + c0, (qt + 1) * TT)
            mm(S_, psm[p % 2][:, c0:TT], Ka[0:99, kc * 128:(kc + 1) * 128], Qa[0:99, tq], True, True,
               [r_Ka[kc // 4], r_Kc, r_Qa[qt], r_Qm[qt], r_Qst[qt], r_Qc], [r_psm[p % 2]])

        def f_P(p):
            qt, i, n = pairs[p]
            kc = mo_kc(p)
            c0, j = cols_of(kc, qt)
            P = Pb[p % 2]
            S_.op("act", lambda e: e.activation(out=P[:, c0:TT], in_=psm[p % 2][:, c0:TT], func=AF.Exp), [r_psm[p % 2]], [r_P[p % 2]])
            if j >= 0:
                mk = cb[:, CB_MLE + 512 * j + c0:CB_MLE + 512 * (j + 1)]
                S_.op("pool", lambda e: e.tensor_tensor(out=P[:, c0:TT], in0=P[:, c0:TT], in1=mk, op=ALU.mult),
                      [r_P[p % 2], r_cb], [r_P[p % 2]])

        def f_PVm(p):
            qt, i, n = pairs[p]
            kc = mo_kc(p)
            c0, j = cols_of(kc, qt)
            P = Pb[p % 2]
            mm(S_, pOa[0:65, c0:TT], Va[:, kc, 0:65], P[:, c0:TT], i == 0, i == n - 1,
               [r_Va[kc // 4], r_Vone, r_P[p % 2]], [r_pOa])
            if i == n - 1:
                tile_fin_mo(qt)

        def rng(p):
            return 0 <= p < NP

        for it in range(-3, NP):
            if rng(it):
                f_W(it)
            if rng(it):
                f_corr(it)
            if rng(it + 1):
                f_R(it + 1)
            if rng(it + 3):
                f_Z(it + 3)
            if rng(it + 2):
                f_ESP(it + 2)
            if rng(it + 2):
                f_Sm(it + 2)
            if rng(it + 1):
                f_P(it + 1)
            if rng(it + 1):
                f_PVm(it + 1)
    S_.finish("sp")


def build_B():
    nc = bass.Bass("TRN2", target_bir_lowering=False)
    hT_all = nc.dram_tensor("hT_all", [NCORES, D, TSH], BF16, kind="ExternalInput").ap()
    wB = nc.dram_tensor("wB", [D, 384], F32, kind="ExternalInput").ap()
    cb_d = nc.dram_tensor("cb", [128, CB_W], BF16, kind="ExternalInput").ap()
    cfB_d = nc.dram_tensor("cfB", [128, 128], F32, kind="ExternalInput").ap()
    kconst_d = nc.dram_tensor("kconst", [35, S], BF16, kind="ExternalInput").ap()
    qconst_d = nc.dram_tensor("qconst", [2, S], BF16, kind="ExternalInput").ap()
    o_loc = nc.dram_tensor("o_loc", [128, NTOK], BF16, kind="ExternalOutput").ap()
    with ExitStack() as stack:
        cx = Ctx(nc, stack)
        phase_B(cx, hT_all, wB, cb_d, cfB_d, kconst_d, qconst_d, o_loc)
        cx.S.emit(nc, stack)
    return nc


def slopes():
    return [2.0 ** (-(h + 1)) for h in range(8)]


def wB_for_head(w_in_l, h):
    cols = []
    for base in (0, 512, 1536, 2048, 1024, 2560):
        cols.append(w_in_l[:, base + 64 * h: base + 64 * (h + 1)])
    return np.ascontiguousarray(np.concatenate(cols, axis=1))


def run_B(hT_all, w_in_l):
    nc = build_B()
    cbn = build_cb()
    sl = slopes()
    in_maps = []
    for h in range(NCORES):
        kc, qc = build_kq_const(sl[h])
        in_maps.append({"hT_all": hT_all, "wB": wB_for_head(w_in_l, h), "cb": cbn, "cfB": build_cfB(sl[h]),
                        "kconst": kc, "qconst": qc})
    res = run_bass_kernel_spmd(nc, in_maps, core_ids=list(range(NCORES)))
    return np.stack([r["o_loc"] for r in res.results], axis=0)


class Job:
    __slots__ = ("load", "compute", "w", "rw")

    def __init__(self, load, compute):
        self.load = load
        self.compute = compute
        self.w = None
        self.rw = None


def run_jobs(jobs, look=2):
    n = len(jobs)
    for i in range(n + look):
        if i < n and jobs[i].load is not None:
            jobs[i].load(jobs[i])
        if i - look >= 0:
            jobs[i - look].compute(jobs[i - look])


class TokState:
    pass


def tok_alloc(cx):
    T = TokState()
    T.xT = cx.sb([128, 8, TSH], F32, "xT")
    T.r_x = [[Res() for _ in range(8)] for _ in range(NSUB)]
    T.ones = cx.sb([128, 128], BF16, "ones")
    T.r_ones = Res()
    T.epsc = cx.sb([128, 1], F32, "epsc")
    T.sqb = cx.sb([128, 8, TT], BF16, "sqb")
    T.r_sq = Res()
    T.rstd = cx.sb([128, TT], F32, "rstd")
    T.r_rstd = Res()
    T.hout = [cx.sb([128, 8, TT], BF16, "hout") for _ in range(2)]
    T.r_hout = mkres(2)
    T.banks = [cx.ps("pc") for _ in range(8)]
    T.r_banks = mkres(8, True)
    T.bank_i = 0
    T.gv = cx.sb([128, 8 * (2 * DEPTH + 1)], F32, "gains")
    T.r_gv = Res()
    return T


def next_bank(T):
    i = T.bank_i % 8
    T.bank_i += 1
    return T.banks[i], T.r_banks[i]


def emit_norm(cx, T, s, gidx, dst, r_dst, out_dt_bf16=True):
    S_ = cx.S
    tsl = slice(s * TT, (s + 1) * TT)
    pt, rp = next_bank(T)
    for k in range(8):
        S_.op("pool", lambda e, k=k: e.tensor_tensor(out=T.sqb[:, k, :], in0=T.xT[:, k, tsl], in1=T.xT[:, k, tsl], op=ALU.mult),
              [T.r_x[s][k]], [T.r_sq])
    for k in range(8):
        mm(S_, pt[:, :], T.ones[:, :], T.sqb[:, k, :], k == 0, k == 7, [T.r_ones, T.r_sq], [rp])
    S_.op("act", lambda e: e.activation(out=T.rstd[:], in_=pt[:, :], func=AF.Sqrt, bias=T.epsc[:, 0:1], scale=1.0 / D),
          [rp, T.r_ones], [T.r_rstd])
    S_.op("dve", lambda e: e.reciprocal(out=T.rstd[:], in_=T.rstd[:]), [T.r_rstd], [T.r_rstd])
    for k in range(8):
        S_.op("dve", lambda e, k=k: e.scalar_tensor_tensor(out=dst[:, k, :], in0=T.xT[:, k, tsl],
                                                           scalar=T.gv[:, gidx * 8 + k:gidx * 8 + k + 1], in1=T.rstd[:],
                                                           op0=ALU.mult, op1=ALU.mult),
              [T.r_x[s][k], T.r_gv, T.r_rstd], [r_dst])


def tok_load_x(cx, T, xT_d, gains_d, cb_d):
    S_ = cx.S
    xv = xT_d.rearrange("(k p) t -> p k t", p=128)
    for k in range(8):
        S_.dma("sp", T.xT[:, k, :], xv[:, k, :], [], [T.r_x[s][k] for s in range(NSUB)])
    S_.dma("sp", T.gv[:], gains_d, [], [T.r_gv])
    S_.dma("sp", T.ones[:], cb_d[:, CB_ONES:CB_ONES + 128], [], [T.r_ones])
    S_.op("pool", lambda e: e.memset(T.epsc[:], EPS), [], [T.r_ones])


def phase_A(cx, T, gidx, hT_out_d):
    S_ = cx.S
    hv = hT_out_d.rearrange("(k p) t -> p k t", p=128)
    for s in range(NSUB):
        ho = T.hout[s % 2]
        rh = T.r_hout[s % 2]
        emit_norm(cx, T, s, gidx, ho, rh)
        S_.dma("sp", hv[:, :, s * TT:(s + 1) * TT], ho[:], [rh], [])


def phase_C(cx, T, W, l, hT_loc_d, hh_d, o_sh_d, final, out_d, CS=None):
    S_ = cx.S
    if CS is None:
        CS = TokState()
        CS.hT = cx.sb([128, 8, TT], BF16, "hT")
        CS.hh = cx.sb([128, 8, 2], BF16, "hh")
        CS.oaT = cx.sb([128, 4, TT], BF16, "oaT")
        CS.obT = cx.sb([128, 4, TT], BF16, "obT")
        CS.ocT = cx.sb([128, 4, TT], BF16, "ocT")
        CS.mT = cx.sb([128, 8, TT], BF16, "mT")
        CS.aT = cx.sb([128, NFC, TT], BF16, "aT")
        CS.wr = [cx.sb([128, NFC * 128], BF16, "wr") for _ in range(4)]
        CS.r_wr = mkres(4)
        CS.wr_i = 0
        CS.gs = [cx.sb([128, TT], F32, "gs") for _ in range(3)]
        CS.r_gs = mkres(3)
        CS.t = [cx.sb([128, TT], F32, "tt") for _ in range(3)]
        CS.r_t = mkres(3)
        CS.xc = cx.sb([128, TT], F32, "xc")
        CS.u = cx.sb([128, TT + 2], F32, "u")
        CS.y = cx.sb([128, TT], F32, "y")
        CS.hu = cx.sb([128, 4, 2], F32, "hu")
        CS.xh = cx.sb([128, 2], F32, "xh")
        CS.sil = cx.sb([128, TT], F32, "sil")
        CS.fout = cx.sb([128, 8, TT], F32, "fout")
        CS.bg = cx.sb([128, 24 * DEPTH], F32, "bg")
        CS.cw = cx.sb([128, 12 * DEPTH], F32, "cw")
        CS.r_hT, CS.r_hh, CS.r_oaT, CS.r_obT, CS.r_ocT, CS.r_mT = Res(), Res(), Res(), Res(), Res(), Res()
        CS.r_aT = mkres(NFC)
        CS.r_xc, CS.r_u, CS.r_y, CS.r_hu, CS.r_xh, CS.r_sil, CS.r_fout, CS.r_bg, CS.r_cw = (Res() for _ in range(9))
        S_.dma("sp", CS.bg[:], W["bg"], [], [CS.r_bg])
        S_.dma("sp", CS.cw[:], W["cw"], [], [CS.r_cw])
    C = CS
    w_in = W["w_in"][l]
    hv = hT_loc_d.rearrange("(k p) t -> p k t", p=128)
    S_.dma("sp", C.hh[:], hh_d, [], [C.r_hh])

    def slab(src, K):
        def load(job):
            i = C.wr_i % 4
            C.wr_i += 1
            view = C.wr[i][:, 0:K * 128].rearrange("p (k n) -> p k n", n=128)
            S_.dma("pool", view, src.rearrange("(k p) n -> p k n", p=128), [], [C.r_wr[i]])
            job.w = view
            job.rw = C.r_wr[i]
        return load

    def slab3(srcs):
        def load(job):
            i = C.wr_i % 4
            C.wr_i += 1
            view = C.wr[i][:, 0:12 * 128].rearrange("p (k n) -> p k n", n=128)
            for q, src in enumerate(srcs):
                S_.dma("pool", view[:, 4 * q:4 * q + 4, :], src.rearrange("(k p) n -> p k n", p=128), [], [C.r_wr[i]])
            job.w = view
            job.rw = C.r_wr[i]
        return load

    def group(pt, rp, wv, koff, K, rhs_of, rreads, ncol=TT):
        for k in range(K):
            mm(S_, pt[:, 0:ncol], wv[:, koff + k, :], rhs_of(k), k == 0, k == K - 1, rreads, [rp])

    jobs = []
    for s in range(NSUB):
        t0 = s * TT
        tsl = slice(t0, t0 + TT)

        def load_acts(job, s=s, t0=t0):
            S_.dma("sp", C.hT[:], hv[:, :, t0:t0 + TT], [], [C.r_hT])
            for kc in range(4):
                for two in range(2):
                    S_.dma("sp", C.oaT[64 * two:64 * two + 64, kc, :], o_sh_d[2 * kc + two, 0:64, t0:t0 + TT], [], [C.r_oaT])
                    S_.dma("sp", C.obT[64 * two:64 * two + 64, kc, :], o_sh_d[2 * kc + two, 64:128, t0:t0 + TT], [], [C.r_obT])
        jobs.append(Job(load_acts, lambda job: None))

        for i in range(4):
            def c_xc(job, s=s, i=i):
                pt, rp = next_bank(T)
                group(pt, rp, job.w, 0, 8, lambda k: C.hT[:, k, :], [job.rw, C.r_hT])
                S_.op("act", lambda e: e.copy(out=C.xc[:], in_=pt[:, :]), [rp], [C.r_xc])
                if s == 0:
                    p2, rp2 = next_bank(T)
                    group(p2, rp2, job.w, 0, 8, lambda k: C.hh[:, k, :], [job.rw, C.r_hh], ncol=2)
                    S_.op("act", lambda e: e.copy(out=C.xh[:], in_=p2[:, 0:2]), [rp2], [C.r_xh])
            jobs.append(Job(slab(w_in[:, 3072 + i * 128:3072 + (i + 1) * 128], 8), c_xc))

            def c_cc(job, s=s, i=i):
                pt, rp = next_bank(T)
                group(pt, rp, job.w, 0, 8, lambda k: C.hT[:, k, :], [job.rw, C.r_hT])
                if s == 0:
                    p2, rp2 = next_bank(T)
                    group(p2, rp2, job.w, 0, 8, lambda k: C.hh[:, k, :], [job.rw, C.r_hh], ncol=2)
                    S_.op("dve", lambda e: e.tensor_tensor(out=C.hu[:, i, :], in0=p2[:, 0:2], in1=C.xh[:], op=ALU.mult),
                          [rp2, C.r_xh], [C.r_hu])
                S_.op("dve", lambda e: e.tensor_copy(out=C.u[:, 0:2], in_=C.hu[:, i, :]), [C.r_hu], [C.r_u])
                S_.op("dve", lambda e: e.tensor_tensor(out=C.u[:, 2:TT + 2], in0=pt[:, :], in1=C.xc[:], op=ALU.mult),
                      [rp, C.r_xc], [C.r_u])
                S_.op("dve", lambda e: e.tensor_copy(out=C.hu[:, i, :], in_=C.u[:, TT:TT + 2]), [C.r_u], [C.r_hu])
                cw0 = l * 12 + i * 3
                S_.op("dve", lambda e: e.tensor_scalar(out=C.y[:], in0=C.u[:, 0:TT], scalar1=C.cw[:, cw0:cw0 + 1], scalar2=None,
                                                       op0=ALU.mult), [C.r_u, C.r_cw], [C.r_y])
                S_.op("dve", lambda e: e.scalar_tensor_tensor(out=C.y[:], in0=C.u[:, 1:TT + 1], scalar=C.cw[:, cw0 + 1:cw0 + 2],
                                                              in1=C.y[:], op0=ALU.mult, op1=ALU.add), [C.r_u, C.r_cw, C.r_y], [C.r_y])
                S_.op("dve", lambda e: e.scalar_tensor_tensor(out=C.y[:], in0=C.u[:, 2:TT + 2], scalar=C.cw[:, cw0 + 2:cw0 + 3],
                                                              in1=C.y[:], op0=ALU.mult, op1=ALU.add), [C.r_u, C.r_cw, C.r_y], [C.r_y])
            jobs.append(Job(slab(w_in[:, 4096 + i * 128:4096 + (i + 1) * 128], 8), c_cc))

            def c_bc(job, i=i):
                pt, rp = next_bank(T)
                group(pt, rp, job.w, 0, 8, lambda k: C.hT[:, k, :], [job.rw, C.r_hT])
                S_.op("dve", lambda e: e.tensor_tensor(out=C.ocT[:, i, :], in0=pt[:, :], in1=C.y[:], op=ALU.mult),
                      [rp, C.r_y], [C.r_ocT])
            jobs.append(Job(slab(w_in[:, 3584 + i * 128:3584 + (i + 1) * 128], 8), c_bc))

        for oc in range(8):
            for br in range(3):
                def c_gate(job, oc=oc, br=br):
                    pt, rp = next_bank(T)
                    group(pt, rp, job.w, 0, 8, lambda k: C.hT[:, k, :], [job.rw, C.r_hT])
                    bcol = l * 24 + br * 8 + oc
                    S_.op("act", lambda e: e.activation(out=C.gs[br][:], in_=pt[:, :], func=AF.Sigmoid,
                                                        bias=C.bg[:, bcol:bcol + 1], scale=1.0),
                          [rp, C.r_bg], [C.r_gs[br]])
                c0 = 4608 + br * 1024 + oc * 128
                jobs.append(Job(slab(w_in[:, c0:c0 + 128], 8), c_gate))

            def c_proj(job, oc=oc):
                srcT = (C.oaT, C.obT, C.ocT)
                rsrc = (C.r_oaT, C.r_obT, C.r_ocT)
                for br in range(3):
                    pt, rp = next_bank(T)
                    group(pt, rp, job.w, 4 * br, 4, lambda k, br=br: srcT[br][:, k, :], [job.rw, rsrc[br]])
                    S_.op("dve", lambda e, br=br, pt=pt: e.tensor_tensor(out=C.t[br][:], in0=pt[:, :], in1=C.gs[br][:], op=ALU.mult),
                          [rp, C.r_gs[br]], [C.r_t[br]])
                S_.op("pool", lambda e: e.tensor_tensor(out=C.t[0][:], in0=C.t[0][:], in1=C.t[1][:], op=ALU.add),
                      [C.r_t[0], C.r_t[1]], [C.r_t[0]])
                S_.op("pool", lambda e: e.tensor_tensor(out=C.mT[:, oc, :], in0=C.t[0][:], in1=C.t[2][:], op=ALU.add),
                      [C.r_t[0], C.r_t[2]], [C.r_mT])
            jobs.append(Job(slab3([W["wpa"][l][:, oc * 128:(oc + 1) * 128], W["wps"][l][:, oc * 128:(oc + 1) * 128],
                                   W["wpc"][l][:, oc * 128:(oc + 1) * 128]]), c_proj))

        for oc in range(8):
            def c_wo(job, s=s, oc=oc, tsl=tsl):
                pt, rp = next_bank(T)
                group(pt, rp, job.w, 0, 8, lambda k: C.mT[:, k, :], [job.rw, C.r_mT])
                S_.op("dve", lambda e: e.tensor_tensor(out=T.xT[:, oc, tsl], in0=pt[:, :], in1=T.xT[:, oc, tsl], op=ALU.add),
                      [rp, T.r_x[s][oc]], [T.r_x[s][oc]])
            jobs.append(Job(slab(W["w_out"][l][:, oc * 128:(oc + 1) * 128], 8), c_wo))

        def c_norm2(job, s=s):
            emit_norm(cx, T, s, 2 * l + 1, C.hT, C.r_hT)
        jobs.append(Job(None, c_norm2))

        for fc in range(NFC):
            def c_fg(job, fc=fc):
                pt, rp = next_bank(T)
                group(pt, rp, job.w, 0, 8, lambda k: C.hT[:, k, :], [job.rw, C.r_hT])
                S_.op("act", lambda e: e.activation(out=C.sil[:], in_=pt[:, :], func=AF.Silu), [rp], [C.r_sil])
            jobs.append(Job(slab(W["wg"][l][:, fc * 128:(fc + 1) * 128], 8), c_fg))

            def c_fu(job, fc=fc):
                pt, rp = next_bank(T)
                group(pt, rp, job.w, 0, 8, lambda k: C.hT[:, k, :], [job.rw, C.r_hT])
                S_.op("dve", lambda e: e.tensor_tensor(out=C.aT[:, fc, :], in0=pt[:, :], in1=C.sil[:], op=ALU.mult),
                      [rp, C.r_sil], [C.r_aT[fc]])
            jobs.append(Job(slab(W["wu"][l][:, fc * 128:(fc + 1) * 128], 8), c_fu))

        for oc in range(8):
            def c_fd(job, s=s, oc=oc, tsl=tsl):
                pt, rp = next_bank(T)
                for k in range(NFC):
                    mm(S_, pt[:, :], job.w[:, k, :], C.aT[:, k, :], k == 0, k == NFC - 1, [job.rw, C.r_aT[k]], [rp])
                S_.op("dve", lambda e: e.tensor_tensor(out=T.xT[:, oc, tsl], in0=pt[:, :], in1=T.xT[:, oc, tsl], op=ALU.add),
                      [rp, T.r_x[s][oc]], [T.r_x[s][oc]])
            jobs.append(Job(slab(W["wd"][l][:, oc * 128:(oc + 1) * 128], NFC), c_fd))

        def c_out(job, s=s, t0=t0):
            if final:
                emit_norm(cx, T, s, 2 * DEPTH, C.fout, C.r_fout)
                S_.dma("sp", out_d.rearrange("(k p) t -> p k t", p=128)[:, :, t0:t0 + TT], C.fout[:], [C.r_fout], [])
            else:
                ho = T.hout[s % 2]
                rh = T.r_hout[s % 2]
                emit_norm(cx, T, s, 2 * (l + 1), ho, rh)
                S_.dma("sp", out_d.rearrange("(k p) t -> p k t", p=128)[:, :, t0:t0 + TT], ho[:], [rh], [])
        jobs.append(Job(None, c_out))
    run_jobs(jobs, look=2)
    return CS


NG = 2 * DEPTH + 1


def declare_weights(nc):
    W = {}
    W["w_in"] = nc.dram_tensor("w_in", [DEPTH, D, INW], F32, kind="ExternalInput").ap()
    W["wpa"] = nc.dram_tensor("wpa", [DEPTH, 512, D], F32, kind="ExternalInput").ap()
    W["wps"] = nc.dram_tensor("wps", [DEPTH, 512, D], F32, kind="ExternalInput").ap()
    W["wpc"] = nc.dram_tensor("wpc", [DEPTH, 512, D], F32, kind="ExternalInput").ap()
    W["w_out"] = nc.dram_tensor("w_out", [DEPTH, D, D], F32, kind="ExternalInput").ap()
    W["wg"] = nc.dram_tensor("wg", [DEPTH, D, DFF], F32, kind="ExternalInput").ap()
    W["wu"] = nc.dram_tensor("wu", [DEPTH, D, DFF], F32, kind="ExternalInput").ap()
    W["wd"] = nc.dram_tensor("wd", [DEPTH, DFF, D], F32, kind="ExternalInput").ap()
    W["bg"] = nc.dram_tensor("bg", [128, 24 * DEPTH], F32, kind="ExternalInput").ap()
    W["cw"] = nc.dram_tensor("cw", [128, 12 * DEPTH], F32, kind="ExternalInput").ap()
    return W


def host_weights(inp):
    f = lambda a: np.ascontiguousarray(np.asarray(a, dtype=np.float32))
    bg = f(inp["b_gate"]).reshape(DEPTH, 24, 128).transpose(2, 0, 1).reshape(128, DEPTH * 24)
    cw = f(inp["conv_w"]).reshape(DEPTH, 3, 4, 128).transpose(3, 0, 2, 1).reshape(128, DEPTH * 12)
    return {"w_in": f(inp["w_in"]), "wpa": f(inp["w_proj_moba"]), "wps": f(inp["w_proj_sb"]), "wpc": f(inp["w_proj_conv"]),
            "w_out": f(inp["w_out"]), "wg": f(inp["w_ffn_gate"]), "wu": f(inp["w_ffn_up"]), "wd": f(inp["w_ffn_down"]),
            "bg": f(bg), "cw": f(cw)}


def host_gains(inp):
    G = [inp["norm_mix_g"][0], inp["norm_ffn_g"][0], inp["norm_mix_g"][1], inp["norm_ffn_g"][1], inp["norm_final_g"]]
    G = np.stack([np.asarray(g, np.float32) for g in G], 0)
    return np.ascontiguousarray(G.reshape(NG, 8, 128).transpose(2, 0, 1).reshape(128, NG * 8))


def build_A():
    nc = bass.Bass("TRN2", target_bir_lowering=False)
    xT_d = nc.dram_tensor("xT", [D, TSH], F32, kind="ExternalInput").ap()
    gains_d = nc.dram_tensor("gains", [128, NG * 8], F32, kind="ExternalInput").ap()
    cb_d = nc.dram_tensor("cb", [128, CB_W], BF16, kind="ExternalInput").ap()
    hT_o = nc.dram_tensor("hT_o", [D, TSH], BF16, kind="ExternalOutput").ap()
    with ExitStack() as stack:
        cx = Ctx(nc, stack)
        T = tok_alloc(cx)
        tok_load_x(cx, T, xT_d, gains_d, cb_d)
        phase_A(cx, T, 0, hT_o)
        cx.S.finish("sp")
        cx.S.emit(nc, stack)
    return nc


def build_C(l, final):
    nc = bass.Bass("TRN2", target_bir_lowering=False)
    xT_d = nc.dram_tensor("xT", [D, TSH], F32, kind="ExternalInput").ap()
    gains_d = nc.dram_tensor("gains", [128, NG * 8], F32, kind="ExternalInput").ap()
    cb_d = nc.dram_tensor("cb", [128, CB_W], BF16, kind="ExternalInput").ap()
    hT_d = nc.dram_tensor("hT_loc", [D, TSH], BF16, kind="ExternalInput").ap()
    hh_d = nc.dram_tensor("hh", [128, 8, 2], BF16, kind="ExternalInput").ap()
    o_sh = nc.dram_tensor("o_sh", [NCORES, 128, TSH], BF16, kind="ExternalInput").ap()
    W = declare_weights(nc)
    if final:
        out_d = nc.dram_tensor("outT", [D, TSH], F32, kind="ExternalOutput").ap()
    else:
        out_d = nc.dram_tensor("hT_o", [D, TSH], BF16, kind="ExternalOutput").ap()
        x2_d = nc.dram_tensor("x2T", [D, TSH], F32, kind="ExternalOutput").ap()
    with ExitStack() as stack:
        cx = Ctx(nc, stack)
        T = tok_alloc(cx)
        tok_load_x(cx, T, xT_d, gains_d, cb_d)
        phase_C(cx, T, W, l, hT_d, hh_d, o_sh, final, out_d)
        if not final:
            xv = x2_d.rearrange("(k p) t -> p k t", p=128)
            for k in range(8):
                cx.S.dma("sp", xv[:, k, :], T.xT[:, k, :], [T.r_x[s][k] for s in range(NSUB)], [])
        cx.S.finish("sp")
        cx.S.emit(nc, stack)
    return nc


def halo_from_hT(hT_sh):
    out = []
    for r in range(NCORES):
        if r % (NCORES // B) == 0:
            hh = np.zeros((D, 2), ml_dtypes.bfloat16)
        else:
            hh = hT_sh[r - 1][:, TSH - 2:TSH]
        out.append(np.ascontiguousarray(hh.reshape(8, 128, 2).transpose(1, 0, 2)))
    return out


def kernel(**inp):
    x = np.asarray(inp["x"], np.float32)
    cores = list(range(NCORES))
    xT_sh = [np.ascontiguousarray(x.reshape(NTOK, D)[r * TSH:(r + 1) * TSH].T) for r in cores]
    gains = host_gains(inp)
    cbn = build_cb()
    Wh = host_weights(inp)
    w_in = np.asarray(inp["w_in"], np.float32)
    res = run_bass_kernel_spmd(build_A(), [{"xT": xT_sh[r], "gains": gains, "cb": cbn} for r in cores], core_ids=cores)
    hT_sh = [res.results[r]["hT_o"] for r in cores]
    out = None
    for l in range(DEPTH):
        hT_all = np.ascontiguousarray(np.stack(hT_sh, 0))
        o_all = run_B(hT_all, w_in[l])
        halos = halo_from_hT(hT_sh)
        final = (l == DEPTH - 1)
        in_maps = []
        for r in cores:
            m = {"xT": xT_sh[r], "gains": gains, "cb": cbn, "hT_loc": hT_sh[r], "hh": halos[r],
                 "o_sh": np.ascontiguousarray(o_all[:, :, r * TSH:(r + 1) * TSH])}
            m.update(Wh)
            in_maps.append(m)
        res = run_bass_kernel_spmd(build_C(l, final), in_maps, core_ids=cores)
        if final:
            out = np.concatenate([res.results[r]["outT"].T for r in cores], axis=0)
        else:
            hT_sh = [res.results[r]["hT_o"] for r in cores]
            xT_sh = [res.results[r]["x2T"] for r in cores]
    return np.ascontiguousarray(out.reshape(B, S, D).astype(np.float32))
```

```python
import numpy as np
import ml_dtypes
from contextlib import ExitStack
import concourse.bass as bass
import concourse.mybir as mybir
from concourse.bass_utils import run_bass_kernel_spmd

F32 = mybir.dt.float32
BF16 = mybir.dt.bfloat16
AF = mybir.ActivationFunctionType
ALU = mybir.AluOpType
AX = mybir.AxisListType

NCORES = 8
D = 1024
B = 2
S = 8192
NTOK = B * S
TSH = NTOK // NCORES
DEPTH = 2
DFF = 2816
NFC = DFF // 128
INW = 7680
EPS = 1e-6
SCALE = 0.125
BIG = 16384.0
TT = 512
NSUB = TSH // TT
NQT = S // TT
NKC = S // 128
NBLK = S // 256
DBG_STOP = 0
HT_CHUNKED = False


class Res:
    __slots__ = ("w", "r", "excl")

    def __init__(self, excl=False):
        self.w = None
        self.r = {}
        self.excl = excl


def mkres(n, excl=False):
    return [Res(excl) for _ in range(n)]


class Sched:
    ENGS = ("pe", "act", "dve", "pool", "sp")
    QSLOTS = {"sp": list(range(0, 8)), "pool": list(range(8, 14)), "act": list(range(14, 16))}

    def __init__(self):
        self.q = {e: [] for e in self.ENGS}
        self.n = {e: 0 for e in self.ENGS}
        self.flag = {e: set() for e in self.ENGS}
        self.waited = {e: {} for e in self.ENGS}
        self.slot_n = [0] * 16
        self.slot_rr = {k: 0 for k in self.QSLOTS}
        self.n_cc = 0
        self.need_pid = set()

    def _need(self, eng, reads, writes):
        need = {}
        for r in reads:
            if r.w is not None:
                k, v = r.w
                if k == eng and eng == "pe":
                    continue
                if v > need.get(k, 0):
                    need[k] = v
            if r.excl:
                for k, v in r.r.items():
                    if k != eng and v > need.get(k, 0):
                        need[k] = v
        for w in writes:
            if w.w is not None:
                k, v = w.w
                if k != eng and v > need.get(k, 0):
                    need[k] = v
            for k, v in w.r.items():
                if k != eng and v > need.get(k, 0):
                    need[k] = v
        return need

    def _waits(self, eng, need):
        wd = self.waited[eng]
        for k, v in need.items():
            if wd.get(k, 0) >= v:
                continue
            wd[k] = v
            if k[0] != "q" and k != "cc":
                self.flag[k].add(v)
            self.q[eng].append(("wait", k, v))

    def op(self, eng, fn, reads=(), writes=()):
        self._waits(eng, self._need(eng, reads, writes))
        self.n[eng] += 1
        idx = self.n[eng]
        self.q[eng].append(("op", fn, idx))
        for r in reads:
            r.r[eng] = idx
        for w in writes:
            w.w = (eng, idx)
            w.r = {}
        return idx

    def dma(self, queue, out, in_, reads=(), writes=()):
        sl = self.QSLOTS[queue]
        slot = sl[self.slot_rr[queue] % len(sl)]
        self.slot_rr[queue] += 1
        key = "q%d" % slot
        need = self._need(None, reads, writes)
        if self.slot_n[slot] > 0:
            need[key] = max(need.get(key, 0), self.slot_n[slot])
        self._waits(queue, need)
        self.slot_n[slot] += 1
        v = self.slot_n[slot]
        self.q[queue].append(("dma", out, in_, slot))
        if callable(out) or callable(in_):
            self.need_pid.add(queue)
        for r in reads:
            r.r[key] = v
        for w in writes:
            w.w = (key, v)
            w.r = {}

    def finish(self, queue):
        need = {}
        for s in range(16):
            if self.slot_n[s] > 0:
                need["q%d" % s] = self.slot_n[s]
        self._waits(queue, need)

    def barrier(self):
        for e in self.ENGS:
            need = {}
            for k in ("pe", "act", "dve", "pool"):
                if k != e and self.n[k] > 0:
                    need[k] = self.n[k]
            for s in range(16):
                if self.slot_n[s] > 0:
                    need["q%d" % s] = self.slot_n[s]
            if self.n_cc > 0:
                need["cc"] = self.n_cc
            self._waits(e, need)

    def coll(self, kind, ins, outs, reads=(), writes=()):
        need = self._need(None, reads, writes)
        self._waits("pool", need)
        self.n_cc += 1
        self.q["pool"].append(("coll", kind, ins, outs))
        for r in reads:
            r.r["cc"] = self.n_cc
        for w in writes:
            w.w = ("cc", self.n_cc)
            w.r = {}

    def emit(self, nc, stack):
        sems = {}
        for k in ("pe", "act", "dve", "pool"):
            sems[k] = stack.enter_context(nc.semaphore("s_" + k))
        for s in range(16):
            sems["q%d" % s] = stack.enter_context(nc.semaphore("s_q%d" % s))
        sems["cc"] = stack.enter_context(nc.semaphore("s_cc"))
        rank = {}
        for k in self.ENGS:
            fl = sorted(self.flag[k])
            rank[k] = {idx: i + 1 for i, idx in enumerate(fl)}
        block = stack.enter_context(nc.Block())
        q = self.q

        need_pid = self.need_pid

        def run(ek, e):
            rk = rank[ek]
            dyn = {}
            if ek in need_pid:
                pid = e.partition_id()
                dyn["pid"] = pid
                dyn["prev"] = (pid + (NCORES - 1)) % NCORES
            for item in q[ek]:
                t = item[0]
                if t == "wait":
                    _, k, v = item
                    val = 16 * v if k[0] == "q" else (v if k == "cc" else rank[k][v])
                    e.wait_ge(sems[k], val)
                elif t == "op":
                    inst = item[1](e)
                    if item[2] in rk:
                        inst.then_inc(sems[ek], 1)
                elif t == "coll":
                    _, kind, ins, outs = item
                    e.collective_compute(kind, ALU.bypass, replica_groups=[list(range(NCORES))], ins=ins, outs=outs).then_inc(sems["cc"])
                else:
                    _, out, in_, slot = item
                    if callable(in_):
                        in_ = in_(dyn)
                    if callable(out):
                        out = out(dyn)
                    e.dma_start(out=out, in_=in_).then_inc(sems["q%d" % slot], 16)

        @block.tensor
        def _(e):
            run("pe", e)

        @block.scalar
        def _(e):
            run("act", e)

        @block.vector
        def _(e):
            run("dve", e)

        @block.gpsimd
        def _(e):
            run("pool", e)

        @block.sync
        def _(e):
            run("sp", e)


class Ctx:
    SB_BASE = 16512
    SB_END = 229376

    def __init__(self, nc, stack):
        self.nc = nc
        self.stack = stack
        self.S = Sched()
        self._n = 0
        self.ptr = self.SB_BASE
        self.peak = self.ptr
        self.banks = None

    def sb(self, shape, dt, name=None):
        self._n += 1
        nbytes = int(np.prod(shape[1:])) * (2 if dt == BF16 else 4)
        nbytes = (nbytes + 31) // 32 * 32
        off = self.ptr
        self.ptr += nbytes
        self.peak = max(self.peak, self.ptr)
        assert self.ptr <= self.SB_END, "SBUF overflow %d" % self.ptr
        return self.nc.alloc_sbuf_tensor_at("%s_%d" % (name or "sb", self._n), list(shape), dt, offset=off)

    def mark(self):
        return self.ptr

    def release(self, m):
        self.ptr = m

    def ps(self, name=None):
        self._n += 1
        return self.stack.enter_context(self.nc.psum_tensor("%s_%d" % (name or "ps", self._n), [128, 512], F32))

    def get_banks(self):
        if self.banks is None:
            self.banks = [self.ps("bank") for _ in range(8)]
        return self.banks


def mm(S, out, lhsT, rhs, start, stop, reads, writes):
    S.op("pe", lambda e: e.matmul(out, lhsT, rhs, start=start, stop=stop, skip_group_check=True), reads, writes)


CB_ONES = 0
CB_IDENT = 128
CB_NTRI = 256
CB_NLOW = 384
CB_MLE = 512
CB_MLT = 512 + 2048
CB_W = 512 + 4096


def build_cb():
    cb = np.zeros((128, CB_W), np.float32)
    cb[:, CB_ONES:CB_ONES + 128] = 1.0
    cb[:, CB_IDENT:CB_IDENT + 128] = np.eye(128)
    jj = np.arange(128)[:, None]
    ss = np.arange(128)[None, :]
    cb[:, CB_NTRI:CB_NTRI + 128] = -1.0 * (jj >= ss)
    cb[:, CB_NLOW:CB_NLOW + 128] = -1.0 * (jj < ss)
    t = np.arange(512)[None, :]
    for j in range(4):
        cb[:, CB_MLE + 512 * j:CB_MLE + 512 * (j + 1)] = (128 * j + jj <= t)
        cb[:, CB_MLT + 512 * j:CB_MLT + 512 * (j + 1)] = (128 * j + jj < t)
    return cb.astype(ml_dtypes.bfloat16)


def build_cfB(slope):
    cf = np.zeros((128, 128), np.float32)
    j = np.arange(64)
    m = np.where(j < 32, 0.0, np.where(j == 32, BIG, -BIG))
    ab = np.where(j <= 32, BIG - slope * 256.0 * (32 - j), 0.0)
    cf[:, 0:64] = m[None, :]
    cf[:, 64:128] = ab[None, :]
    return cf


def build_kq_const(slope):
    s = np.arange(S)
    kc = np.zeros((35, S), np.float32)
    kc[s // 256, s] = 1.0
    kc[32, :] = 1.0
    kc[33, :] = slope * (s % 256)
    kc[34, :] = 1.0
    qc = np.zeros((2, S), np.float32)
    qc[0, :] = 1.0
    qc[1, :] = -slope * (s % 256)
    return kc.astype(ml_dtypes.bfloat16), qc.astype(ml_dtypes.bfloat16)


def phase_B(cx, hT_all, wB, cb_d, cfB_d, kconst_d, qconst_d, o_loc, r_hT_all=None, r_o_loc=None, do_finish=True, shard_done=None):
    S_ = cx.S
    cb = cx.sb([128, CB_W], BF16, "cb")
    cf = cx.sb([128, 128], F32, "cfB")
    onesf = cx.sb([128, 64], F32, "onesf")
    wq = cx.sb([128, 8, 384], BF16, "wq")
    Qa = cx.sb([128, S], BF16, "Qa")
    Ka = cx.sb([128, S], BF16, "Ka")
    Va = cx.sb([128, NKC, 65], BF16, "Va")
    Qs = cx.sb([128, S], BF16, "Qs")
    Ks = cx.sb([128, S], BF16, "Ks")
    Vs = cx.sb([128, NKC, 64], BF16, "Vs")
    hbuf = [cx.sb([128, 8, TT], BF16, "hb") for _ in range(2)]
    kmean = cx.sb([128, 32], F32, "kmean")
    kmean_b = cx.sb([128, 32], BF16, "kmeanb")
    kmaxt = cx.sb([128, NQT], F32, "kmaxt")
    negc = cx.sb([128, 1], F32, "negc")
    sq = cx.sb([128, TT], BF16, "sq")
    gate = cx.sb([128, 32], F32, "gate")
    top8 = cx.sb([128, 8], F32, "top8")
    thr = cx.sb([128, 1], F32, "thr")
    sel = cx.sb([128, 32], F32, "sel")
    mpad = [cx.sb([128, 96], BF16, "mpad") for _ in range(2)]
    nqb = [cx.sb([128, TT], BF16, "nq") for _ in range(2)]
    Ebuf = [cx.sb([128, TT], F32, "E") for _ in range(4)]
    Xbuf = [cx.sb([128, TT], F32, "X") for _ in range(2)]
    r_X = mkres(2)
    SPb = [cx.sb([128, TT], BF16, "SP") for _ in range(3)]
    Wb = [cx.sb([128, TT], BF16, "W") for _ in range(2)]
    Pb = [cx.sb([128, TT], BF16, "P") for _ in range(2)]
    rl = cx.sb([128, TT], F32, "rl")
    oaf = cx.sb([128, TT], F32, "oaf")
    oout = [cx.sb([128, TT], BF16, "oout") for _ in range(2)]
    bk = cx.get_banks()
    pz = bk[0:2]
    psm = bk[2:4]
    pR, pOs, pOa, pX = bk[4], bk[5], bk[6], bk[7]
    r_hT_all = r_hT_all if r_hT_all is not None else Res()
    r_o_loc = r_o_loc if r_o_loc is not None else Res()

    def r_o_of(g):
        return r_o_loc[g // TSH] if isinstance(r_o_loc, list) else r_o_loc

    r_cb, r_cf, r_onesf, r_wq = Res(), Res(), Res(), Res()
    r_Qa = mkres(NQT)
    r_Qm = mkres(NQT)
    r_Qst = mkres(NQT)
    r_Qc = Res()
    r_Ka = mkres(NQT)
    r_Kc = Res()
    r_Va = mkres(NQT)
    r_Vone = Res()
    r_Qs = mkres(NQT)
    r_Ks = mkres(NQT)
    r_Vs = mkres(NQT)
    r_hb = mkres(2)
    r_kmean, r_kmeanb, r_kmaxt, r_negc, r_sq = Res(), Res(), Res(), Res(), Res()
    r_gate, r_top8, r_thr, r_sel = Res(), Res(), Res(), Res()
    r_mpad = mkres(2)
    r_nq = mkres(2)
    r_E = mkres(4)
    r_SP = mkres(3)
    r_W = mkres(2)
    r_P = mkres(2)
    r_rl, r_oaf = Res(), Res()
    r_oout = mkres(2)
    r_pz = mkres(2, True)
    r_psm = mkres(2, True)
    r_pR, r_pOs, r_pOa, r_pX = Res(True), Res(True), Res(True), Res(True)

    S_.dma("sp", cb[:], cb_d, [], [r_cb])
    S_.dma("sp", cf[:], cfB_d, [], [r_cf])
    S_.dma("pool", wq[:], wB.rearrange("(k p) n -> p k n", p=128), [], [r_wq])
    S_.op("pool", lambda e: e.memset(onesf[:], 1.0), [], [r_onesf])
    S_.op("pool", lambda e: e.memset(kmean[:], 0.0), [], [r_kmean])
    S_.op("pool", lambda e: e.memset(Va[:, :, 64:65], 1.0), [], [r_Vone])
    for i in range(2):
        S_.op("pool", lambda e, i=i: e.memset(mpad[i][:], 0.0), [], [r_mpad[i]])

    if DBG_STOP == 10:
        S_.dma("sp", o_loc[:, 0:384], wq[:, 0, :], [r_wq, r_cb, r_cf, r_onesf, r_Vone, r_kmean] + r_mpad, [])
        S_.finish("sp")
        return
    ones_bf = cb[:, CB_ONES:CB_ONES + 128]
    ident = cb[:, CB_IDENT:CB_IDENT + 128]
    ntri = cb[:, CB_NTRI:CB_NTRI + 128]
    nlow = cb[:, CB_NLOW:CB_NLOW + 128]

    for b in range(B):
        if b == 0:
            S_.dma("sp", Ka[64:99, :], kconst_d, [], [r_Kc])
            S_.dma("sp", Qa[97:99, :], qconst_d, [], [r_Qc])
        for j in range(NQT):
            hb = hbuf[j % 2]
            rh = r_hb[j % 2]
            rank, tl = divmod(b * S + j * TT, TSH)
            if HT_CHUNKED:
                hsrc = hT_all[:, rank, :, tl:tl + TT].rearrange("k p t -> p k t")
            else:
                hsrc = hT_all[rank, :, tl:tl + TT].rearrange("(k p) t -> p k t", p=128)
            S_.dma("sp", hb[:], hsrc, [r_hT_all], [rh])
            tsl = slice(j * TT, (j + 1) * TT)
            tgt = [(pz[0], r_pz[0], 0), (pz[1], r_pz[1], 64), (psm[0], r_psm[0], 128), (psm[1], r_psm[1], 192)]
            for (pt, rp, c0) in tgt:
                for k in range(8):
                    mm(S_, pt[0:64, :], wq[:, k, c0:c0 + 64], hb[:, k, :], k == 0, k == 7, [r_wq, rh], [rp])
            for c in range(4):
                for k in range(8):
                    mm(S_, pX[:, c * 128:(c + 1) * 128], hb[:, k, c * 128:(c + 1) * 128], wq[:, k, 256:384],
                       k == 0, k == 7, [r_wq, rh], [r_pX])
            if DBG_STOP == 11:
                S_.op("dve", lambda e: e.tensor_copy(out=Ka[0:64, 0:512], in_=pz[1][0:64, :]), [r_pz[1]], [r_Ka[j]])
                S_.op("dve", lambda e: e.tensor_copy(out=Qa[0:64, 0:512], in_=pX[0:64, :]), [r_pX, r_pz[0], r_psm[0], r_psm[1]], [r_Qa[j]])
                S_.dma("sp", o_loc[0:64, 0:512], Ka[0:64, 0:512], [r_Ka[j], r_Qa[j]], [])
                S_.finish("sp")
                return
            S_.op("act", lambda e, tsl=tsl: e.mul(out=Qa[0:64, tsl], in_=pz[0][0:64, :], mul=SCALE),
                  [r_pz[0]], [r_Qa[j]])
            S_.op("dve", lambda e, tsl=tsl: e.tensor_copy(out=Ka[0:64, tsl], in_=pz[1][0:64, :]), [r_pz[1]], [r_Ka[j]])
            S_.op("dve", lambda e, j=j: e.tensor_reduce(out=kmean[0:64, 2 * j:2 * j + 2],
                                                        in_=pz[1][0:64, :].rearrange("p (a b) -> p a b", b=256),
                                                        axis=AX.X, op=ALU.add), [r_pz[1]], [r_kmean])
            S_.op("act", lambda e, tsl=tsl: e.mul(out=Qs[0:64, tsl], in_=psm[0][0:64, :], mul=SCALE),
                  [r_psm[0]], [r_Qs[j]])
            S_.op("dve", lambda e, tsl=tsl: e.tensor_copy(out=Ks[0:64, tsl], in_=psm[1][0:64, :]), [r_psm[1]], [r_Ks[j]])
            pXv = pX[:].rearrange("p (c n) -> p c n", n=128)
            S_.op("act", lambda e, j=j, pXv=pXv: e.copy(out=Va[:, 4 * j:4 * j + 4, 0:64], in_=pXv[:, :, 0:64]),
                  [r_pX], [r_Va[j]])
            S_.op("dve", lambda e, j=j, pXv=pXv: e.tensor_copy(out=Vs[:, 4 * j:4 * j + 4, :], in_=pXv[:, :, 64:128]),
                  [r_pX], [r_Vs[j]])
            if DBG_STOP == 12:
                S_.dma("sp", o_loc[0:64, 0:512], Ka[0:64, 0:512], [r_Ka[j], r_Qa[j], r_Qs[j], r_Ks[j], r_Va[j], r_Vs[j], r_kmean], [])
                S_.finish("sp")
                return
            S_.op("pool", lambda e, tsl=tsl: e.tensor_tensor(out=sq[0:64, :], in0=Ka[0:64, tsl], in1=Ka[0:64, tsl], op=ALU.mult),
                  [r_Ka[j]], [r_sq])
            mm(S_, pR[0:97, :], ones_bf[0:64, 0:97], sq[0:64, :], True, True, [r_cb, r_sq], [r_pR])
            S_.op("dve", lambda e, j=j: e.tensor_reduce(out=kmaxt[0:97, j:j + 1], in_=pR[0:97, :], axis=AX.X, op=ALU.max),
                  [r_pR], [r_kmaxt])
        if DBG_STOP == 13:
            S_.dma("sp", o_loc[0:64, 0:512], Ka[0:64, 0:512], [r_Ka[0], r_kmaxt], [])
            S_.finish("sp")
            return
        S_.op("dve", lambda e: e.tensor_reduce(out=negc[0:97, :], in_=kmaxt[0:97, :], axis=AX.X, op=ALU.max), [r_kmaxt], [r_negc])
        S_.op("dve", lambda e: e.tensor_scalar(out=negc[0:97, :], in0=negc[0:97, :], scalar1=-0.5 * SCALE, scalar2=None, op0=ALU.mult),
              [r_negc], [r_negc])
        S_.op("dve", lambda e: e.tensor_copy(out=kmean_b[0:64, :], in_=kmean[0:64, :]), [r_kmean], [r_kmeanb])
        if DBG_STOP == 1:
            S_.dma("sp", o_loc[0:64, 0:S], Qa[0:64, :], r_Qa, [])
            S_.dma("sp", o_loc[64:128, 0:S], Ks[0:64, :], r_Ks, [])
            break
        for c in range(S // 128):
            blk = c // 2
            jt = c // 4
            csl = slice(c * 128, (c + 1) * 128)
            mp = mpad[c % 2]
            rmp = r_mpad[c % 2]
            mm(S_, pX[:, 0:32], Qa[0:64, csl], kmean_b[0:64, :], True, True, [r_Qa[jt], r_kmeanb], [r_pX])
            S_.op("dve", lambda e, blk=blk: e.tensor_tensor(out=gate[:], in0=pX[:, 0:32], in1=cf[:, 32 - blk:64 - blk], op=ALU.add),
                  [r_pX, r_cf], [r_gate])
            S_.op("dve", lambda e: e.max(out=top8[:], in_=gate[:]), [r_gate], [r_top8])
            S_.op("dve", lambda e: e.tensor_scalar(out=thr[:], in0=top8[:, 3:4], scalar1=-BIG / 2, scalar2=None, op0=ALU.max),
                  [r_top8], [r_thr])
            S_.op("dve", lambda e: e.tensor_scalar(out=sel[:], in0=gate[:], scalar1=thr[:, 0:1], scalar2=None, op0=ALU.is_ge),
                  [r_gate, r_thr], [r_sel])
            S_.op("dve", lambda e, blk=blk: e.tensor_tensor(out=sel[:], in0=sel[:], in1=cf[:, 64 + 32 - blk:64 + 64 - blk], op=ALU.mult),
                  [r_sel, r_cf], [r_sel])
            S_.op("dve", lambda e, mp=mp: e.tensor_scalar(out=mp[:, 64:96], in0=sel[:], scalar1=-BIG, scalar2=None, op0=ALU.add),
                  [r_sel], [rmp])
            mm(S_, psm[c % 2][0:96, 0:128], mp[:, 0:96], ident, True, True, [rmp, r_cb], [r_psm[c % 2]])
            S_.op("act", lambda e, c=c, csl=csl: e.copy(out=Qa[64:96, csl], in_=psm[c % 2][64:96, 0:128]),
                  [r_psm[c % 2]], [r_Qm[jt]])
        if DBG_STOP == 2:
            S_.dma("sp", o_loc[0:99, 0:S], Qa[0:99, :], r_Qa + r_Qm + [r_Qc], [])
            break
        pairs = []
        for qt in range(NQT):
            n = 4 * qt + 4
            for i in range(n):
                pairs.append((qt, i, n))
        NP = len(pairs)

        def cols_of(kc, qt):
            j = kc - 4 * qt
            return (128 * j if j >= 0 else 0), j

        def sb_kc(p):
            qt, i, n = pairs[p]
            return n - 1 - i

        def mo_kc(p):
            qt, i, n = pairs[p]
            return i

        def tile_init(qt):
            tsl = slice(qt * TT, (qt + 1) * TT)
            S_.op("dve", lambda e: e.tensor_tensor(out=sq[0:64, :], in0=Qa[0:64, tsl], in1=Qa[0:64, tsl], op=ALU.mult),
                  [r_Qa[qt]], [r_sq])
            mm(S_, pX[0:97, :], ones_bf[0:64, 0:97], sq[0:64, :], True, True, [r_cb, r_sq], [r_pX])
            S_.op("dve", lambda e: e.tensor_scalar(out=Qa[96:97, tsl], in0=pX[96:97, :], scalar1=-0.5 / SCALE,
                                                   scalar2=negc[96:97, 0:1], op0=ALU.mult, op1=ALU.add),
                  [r_pX, r_negc], [r_Qst[qt]])
            nq = nqb[qt % 2]
            S_.op("pool", lambda e: e.tensor_scalar(out=nq[0:64, :], in0=Qs[0:64, tsl], scalar1=-1.0, scalar2=None, op0=ALU.mult),
                  [r_Qs[qt]], [r_nq[qt % 2]])

        def o_dst(qt, lo):
            g = b * S + qt * TT
            if HT_CHUNKED:
                c, off = divmod(g, TSH)
                return o_loc[c, lo:lo + 64, off:off + TT]
            return o_loc[lo:lo + 64, g:g + TT]

        def tile_fin_sb(qt):
            ob = oout[1]
            S_.op("dve", lambda e: e.tensor_copy(out=ob[0:64, :], in_=pOs[0:64, :]), [r_pOs], [r_oout[1]])
            g = b * S + qt * TT
            S_.dma("sp", o_dst(qt, 64), ob[0:64, :], [r_oout[1]], [r_o_of(g)])
            if shard_done is not None and (g + TT) % TSH == 0:
                shard_done(g // TSH)

        def tile_fin_mo(qt):
            ob = oout[0]
            S_.op("dve", lambda e: e.reciprocal(out=rl[64:65, :], in_=pOa[64:65, :]), [r_pOa], [r_rl])
            S_.op("act", lambda e: e.copy(out=oaf[0:64, :], in_=pOa[0:64, :]), [r_pOa], [r_oaf])
            mm(S_, pX[0:64, :], onesf[64:65, 0:64], rl[64:65, :], True, True, [r_onesf, r_rl], [r_pX])
            S_.op("dve", lambda e: e.tensor_tensor(out=ob[0:64, :], in0=oaf[0:64, :], in1=pX[0:64, :], op=ALU.mult),
                  [r_oaf, r_pX], [r_oout[0]])
            S_.dma("sp", o_dst(qt, 0), ob[0:64, :], [r_oout[0]], [r_o_of(b * S + qt * TT)])

        def f_Z(p):
            qt, i, n = pairs[p]
            if i == 0:
                tile_init(qt)
            kc = sb_kc(p)
            c0, j = cols_of(kc, qt)
            tq = slice(qt * TT + c0, (qt + 1) * TT)
            mm(S_, pz[p % 2][:, c0:TT], Ks[0:64, kc * 128:(kc + 1) * 128], Qs[0:64, tq], True, True,
               [r_Ks[kc // 4], r_Qs[qt]], [r_pz[p % 2]])

        def f_E(p):
            qt, i, n = pairs[p]
            kc = sb_kc(p)
            c0, j = cols_of(kc, qt)
            E = Ebuf[p % 4]
            S_.op("act", lambda e: e.activation(out=E[:, c0:TT], in_=pz[p % 2][:, c0:TT], func=AF.Exp), [r_pz[p % 2]], [r_E[p % 4]])

        def f_SP(p):
            qt, i, n = pairs[p]
            kc = sb_kc(p)
            c0, j = cols_of(kc, qt)
            E = Ebuf[p % 4]
            SP = SPb[p % 3]
            S_.op("act", lambda e: e.activation(out=SP[:, c0:TT], in_=E[:, c0:TT], func=AF.Ln, bias=1.0), [r_E[p % 4]], [r_SP[p % 3]])
            if j >= 0:
                mk = cb[:, CB_MLT + 512 * j + c0:CB_MLT + 512 * (j + 1)]
                S_.op("pool", lambda e: e.tensor_tensor(out=SP[:, c0:TT], in0=SP[:, c0:TT], in1=mk, op=ALU.mult),
                      [r_SP[p % 3], r_cb], [r_SP[p % 3]])

        def f_R(p):
            qt, i, n = pairs[p]
            kc = sb_kc(p)
            c0, j = cols_of(kc, qt)
            SP = SPb[p % 3]
            mm(S_, pR[:, c0:TT], ntri, SP[:, c0:TT], i == 0, True, [r_cb, r_SP[p % 3]], [r_pR])

        def f_W(p):
            qt, i, n = pairs[p]
            kc = sb_kc(p)
            c0, j = cols_of(kc, qt)
            W = Wb[p % 2]
            X = Xbuf[p % 2]
            E = Ebuf[p % 4]
            S_.op("act", lambda e: e.activation(out=X[:, c0:TT], in_=pR[:, c0:TT], func=AF.Exp), [r_pR], [r_X[p % 2]])
            S_.op("dve", lambda e: e.tensor_tensor(out=W[:, c0:TT], in0=E[:, c0:TT], in1=X[:, c0:TT], op=ALU.mult),
                  [r_E[p % 4], r_X[p % 2]], [r_W[p % 2]])
            if j >= 0:
                mk = cb[:, CB_MLT + 512 * j + c0:CB_MLT + 512 * (j + 1)]
                S_.op("pool", lambda e: e.tensor_tensor(out=W[:, c0:TT], in0=W[:, c0:TT], in1=mk, op=ALU.mult),
                      [r_W[p % 2], r_cb], [r_W[p % 2]])

        def f_corr(p):
            qt, i, n = pairs[p]
            kc = sb_kc(p)
            c0, j = cols_of(kc, qt)
            SP = SPb[p % 3]
            W = Wb[p % 2]
            if i < n - 1:
                mm(S_, pR[:, c0:TT], nlow, SP[:, c0:TT], False, True, [r_cb, r_SP[p % 3]], [r_pR])
            mm(S_, pOs[0:64, c0:TT], Vs[:, kc, :], W[:, c0:TT], i == 0, i == n - 1, [r_Vs[kc // 4], r_W[p % 2]], [r_pOs])
            if i == n - 1:
                tile_fin_sb(qt)

        def f_Sm(p):
            qt, i, n = pairs[p]
            kc = mo_kc(p)
            c0, j = cols_of(kc, qt)
            tq = slice(qt * TT + c0, (qt + 1) * TT)
            mm(S_, psm[p % 2][:, c0:TT], Ka[0:99, kc * 128:(kc + 1) * 128], Qa[0:99, tq], True, True,
               [r_Ka[kc // 4], r_Kc, r_Qa[qt], r_Qm[qt], r_Qst[qt], r_Qc], [r_psm[p % 2]])

        def f_P(p):
            qt, i, n = pairs[p]
            kc = mo_kc(p)
            c0, j = cols_of(kc, qt)
            P = Pb[p % 2]
            S_.op("act", lambda e: e.activation(out=P[:, c0:TT], in_=psm[p % 2][:, c0:TT], func=AF.Exp), [r_psm[p % 2]], [r_P[p % 2]])
            if j >= 0:
                mk = cb[:, CB_MLE + 512 * j + c0:CB_MLE + 512 * (j + 1)]
                S_.op("pool", lambda e: e.tensor_tensor(out=P[:, c0:TT], in0=P[:, c0:TT], in1=mk, op=ALU.mult),
                      [r_P[p % 2], r_cb], [r_P[p % 2]])

        def f_PVm(p):
            qt, i, n = pairs[p]
            kc = mo_kc(p)
            c0, j = cols_of(kc, qt)
            P = Pb[p % 2]
            mm(S_, pOa[0:65, c0:TT], Va[:, kc, 0:65], P[:, c0:TT], i == 0, i == n - 1,
               [r_Va[kc // 4], r_Vone, r_P[p % 2]], [r_pOa])
            if i == n - 1:
                tile_fin_mo(qt)

        def rng(p):
            return 0 <= p < NP

        for it in range(-3, NP):
            if rng(it):
                f_W(it)
            if rng(it):
                f_corr(it)
            if rng(it + 1):
                f_R(it + 1)
            if rng(it + 3):
                f_Z(it + 3)
            if rng(it + 2):
                f_E(it + 2)
            if rng(it + 2):
                f_Sm(it + 2)
            if rng(it + 1):
                f_P(it + 1)
            if rng(it + 2):
                f_SP(it + 2)
            if rng(it + 1):
                f_PVm(it + 1)
    if do_finish:
        S_.finish("sp")


def build_B():
    nc = bass.Bass("TRN2", target_bir_lowering=False)
    hT_all = nc.dram_tensor("hT_all", [NCORES, D, TSH], BF16, kind="ExternalInput").ap()
    wB = nc.dram_tensor("wB", [D, 384], F32, kind="ExternalInput").ap()
    cb_d = nc.dram_tensor("cb", [128, CB_W], BF16, kind="ExternalInput").ap()
    cfB_d = nc.dram_tensor("cfB", [128, 128], F32, kind="ExternalInput").ap()
    kconst_d = nc.dram_tensor("kconst", [35, S], BF16, kind="ExternalInput").ap()
    qconst_d = nc.dram_tensor("qconst", [2, S], BF16, kind="ExternalInput").ap()
    o_loc = nc.dram_tensor("o_loc", [128, NTOK], BF16, kind="ExternalOutput").ap()
    with ExitStack() as stack:
        cx = Ctx(nc, stack)
        phase_B(cx, hT_all, wB, cb_d, cfB_d, kconst_d, qconst_d, o_loc)
        cx.S.emit(nc, stack)
    return nc


def slopes():
    return [2.0 ** (-(h + 1)) for h in range(8)]


def wB_for_head(w_in_l, h):
    cols = []
    for base in (0, 512, 1536, 2048, 1024, 2560):
        cols.append(w_in_l[:, base + 64 * h: base + 64 * (h + 1)])
    return np.ascontiguousarray(np.concatenate(cols, axis=1))


def run_B(hT_all, w_in_l):
    nc = build_B()
    cbn = build_cb()
    sl = slopes()
    in_maps = []
    for h in range(NCORES):
        kc, qc = build_kq_const(sl[h])
        in_maps.append({"hT_all": hT_all, "wB": wB_for_head(w_in_l, h), "cb": cbn, "cfB": build_cfB(sl[h]),
                        "kconst": kc, "qconst": qc})
    res = run_bass_kernel_spmd(nc, in_maps, core_ids=list(range(NCORES)))
    return np.stack([r["o_loc"] for r in res.results], axis=0)


class Job:
    __slots__ = ("load", "compute", "w", "rw")

    def __init__(self, load, compute):
        self.load = load
        self.compute = compute
        self.w = None
        self.rw = None


def run_jobs(jobs, look=2):
    n = len(jobs)
    for i in range(n + look):
        if i < n and jobs[i].load is not None:
            jobs[i].load(jobs[i])
        if i - look >= 0:
            jobs[i - look].compute(jobs[i - look])


class TokState:
    pass


def tok_alloc(cx):
    T = TokState()
    T.xT = cx.sb([128, 8, TSH], F32, "xT")
    T.r_x = [[Res() for _ in range(8)] for _ in range(NSUB)]
    T.ones = cx.sb([128, 128], BF16, "ones")
    T.r_ones = Res()
    T.epsc = cx.sb([128, 1], F32, "epsc")
    T.sqb = cx.sb([128, 8, TT], BF16, "sqb")
    T.r_sq = Res()
    T.rstd = cx.sb([128, TT], F32, "rstd")
    T.r_rstd = Res()
    T.hout = [cx.sb([128, 8, TT], BF16, "hout") for _ in range(2)]
    T.r_hout = mkres(2)
    T.banks = cx.get_banks()
    T.r_banks = mkres(8, True)
    T.bank_i = 0
    T.gv = cx.sb([128, 8 * (2 * DEPTH + 1)], F32, "gains")
    T.r_gv = Res()
    return T


def next_bank(T):
    i = T.bank_i % 8
    T.bank_i += 1
    return T.banks[i], T.r_banks[i]


def emit_norm(cx, T, s, gidx, dst, r_dst, out_dt_bf16=True):
    S_ = cx.S
    tsl = slice(s * TT, (s + 1) * TT)
    pt, rp = next_bank(T)
    for k in range(8):
        S_.op("pool", lambda e, k=k: e.tensor_tensor(out=T.sqb[:, k, :], in0=T.xT[:, k, tsl], in1=T.xT[:, k, tsl], op=ALU.mult),
              [T.r_x[s][k]], [T.r_sq])
    for k in range(8):
        mm(S_, pt[:, :], T.ones[:, :], T.sqb[:, k, :], k == 0, k == 7, [T.r_ones, T.r_sq], [rp])
    S_.op("act", lambda e: e.activation(out=T.rstd[:], in_=pt[:, :], func=AF.Sqrt, bias=T.epsc[:, 0:1], scale=1.0 / D),
          [rp, T.r_ones], [T.r_rstd])
    S_.op("dve", lambda e: e.reciprocal(out=T.rstd[:], in_=T.rstd[:]), [T.r_rstd], [T.r_rstd])
    for k in range(8):
        S_.op("dve", lambda e, k=k: e.scalar_tensor_tensor(out=dst[:, k, :], in0=T.xT[:, k, tsl],
                                                           scalar=T.gv[:, gidx * 8 + k:gidx * 8 + k + 1], in1=T.rstd[:],
                                                           op0=ALU.mult, op1=ALU.mult),
              [T.r_x[s][k], T.r_gv, T.r_rstd], [r_dst])


def tok_load_x(cx, T, xT_d, gains_d, cb_d, r_src=None):
    S_ = cx.S
    xv = xT_d.rearrange("(k p) t -> p k t", p=128)
    for k in range(8):
        S_.dma("sp", T.xT[:, k, :], xv[:, k, :], [r_src] if r_src else [], [T.r_x[s][k] for s in range(NSUB)])
    S_.dma("sp", T.gv[:], gains_d, [], [T.r_gv])
    S_.dma("sp", T.ones[:], cb_d[:, CB_ONES:CB_ONES + 128], [], [T.r_ones])
    S_.op("pool", lambda e: e.memset(T.epsc[:], EPS), [], [T.r_ones])


def phase_A(cx, T, gidx, hT_out_d, r_hT_loc=None):
    S_ = cx.S
    hv = hT_out_d.rearrange("(k p) t -> p k t", p=128)
    for s in range(NSUB):
        ho = T.hout[s % 2]
        rh = T.r_hout[s % 2]
        emit_norm(cx, T, s, gidx, ho, rh)
        S_.dma("sp", hv[:, :, s * TT:(s + 1) * TT], ho[:], [rh], [r_hT_loc[s]] if r_hT_loc else [])


def phase_C(cx, T, W, l, hT_loc_d, hh_d, o_sh_d, final, out_d, CS=None, dyn=None):
    S_ = cx.S
    if CS is None:
        CS = TokState()
        CS.hT = cx.sb([128, 8, TT], BF16, "hT")
        CS.hh = cx.sb([128, 8, 2], BF16, "hh")
        CS.ost = cx.sb([128, 8, TT], BF16, "ost")
        CS.r_ost = Res()
        CS.ocT = cx.sb([128, 4, TT], BF16, "ocT")
        CS.mT = cx.sb([128, 8, TT], BF16, "mT")
        CS.aT = cx.sb([128, NFC, TT], BF16, "aT")
        CS.wr = [cx.sb([128, NFC * 128], BF16, "wr") for _ in range(4)]
        CS.r_wr = mkres(4)
        CS.wr_i = 0
        CS.gs = [cx.sb([128, TT], F32, "gs") for _ in range(3)]
        CS.r_gs = mkres(3)
        CS.t = [cx.sb([128, TT], F32, "tt") for _ in range(3)]
        CS.r_t = mkres(3)
        CS.xc = cx.sb([128, TT], F32, "xc")
        CS.u = cx.sb([128, TT + 2], F32, "u")
        CS.y = cx.sb([128, TT], F32, "y")
        CS.hu = cx.sb([128, 4, 2], F32, "hu")
        CS.xh = cx.sb([128, 2], F32, "xh")
        CS.sil = cx.sb([128, TT], F32, "sil")
        CS.fout = cx.sb([128, 8, TT], F32, "fout")
        CS.bg = cx.sb([128, 24 * DEPTH], F32, "bg")
        CS.cw = cx.sb([128, 12 * DEPTH], F32, "cw")
        CS.r_hT, CS.r_hh, CS.r_ocT, CS.r_mT = Res(), Res(), Res(), Res()
        CS.r_aT = mkres(NFC)
        CS.r_xc, CS.r_u, CS.r_y, CS.r_hu, CS.r_xh, CS.r_sil, CS.r_fout, CS.r_bg, CS.r_cw = (Res() for _ in range(9))
        S_.dma("sp", CS.bg[:], W["bg"], [], [CS.r_bg])
        S_.dma("sp", CS.cw[:], W["cw"], [], [CS.r_cw])
    C = CS
    w_in = W["w_in"][l]
    hv = hT_loc_d.rearrange("(k p) t -> p k t", p=128)
    if dyn is None:
        S_.dma("sp", C.hh[:], hh_d, [], [C.r_hh])
        r_hl = [Res() for _ in range(NSUB)]
    else:
        r_hl = dyn["r_hT_loc"]
        hhraw = cx.sb([128, 8, 2], BF16, "hhraw")
        hm = cx.sb([128, 1], F32, "hm")
        r_hhraw, r_hm = Res(), Res()
        hT_all3 = dyn["hT_all3"]
        S_.dma("sp", hhraw[:], lambda d: hT_all3[:, bass.ds(d["prev"], 1), :, TSH - 2:TSH].rearrange("k a p t -> p (k a) t"),
               [dyn["r_hT_all"]], [r_hhraw])
        S_.dma("sp", hm[:], dyn["hmask"], [], [r_hm])
        S_.op("dve", lambda e: e.tensor_scalar(out=C.hh[:], in0=hhraw[:], scalar1=hm[:, 0:1], scalar2=None, op0=ALU.mult),
              [r_hhraw, r_hm], [C.r_hh])

    def slab(src, K):
        def load(job):
            i = C.wr_i % 4
            C.wr_i += 1
            view = C.wr[i][:, 0:K * 128].rearrange("p (k n) -> p k n", n=128)
            S_.dma("pool", view, src.rearrange("(k p) n -> p k n", p=128), [], [C.r_wr[i]])
            job.w = view
            job.rw = C.r_wr[i]
        return load

    def slab3(srcs):
        def load(job):
            i = C.wr_i % 4
            C.wr_i += 1
            view = C.wr[i][:, 0:12 * 128].rearrange("p (k n) -> p k n", n=128)
            S_.dma("pool", view[0:64, 0:8, :], srcs[0].rearrange("(h d) n -> d h n", d=64), [], [C.r_wr[i]])
            S_.dma("pool", view[64:128, 0:8, :], srcs[1].rearrange("(h d) n -> d h n", d=64), [], [C.r_wr[i]])
            S_.dma("pool", view[:, 8:12, :], srcs[2].rearrange("(k p) n -> p k n", p=128), [], [C.r_wr[i]])
            job.w = view
            job.rw = C.r_wr[i]
        return load

    def group(pt, rp, wv, koff, K, rhs_of, rreads, ncol=TT):
        for k in range(K):
            mm(S_, pt[:, 0:ncol], wv[:, koff + k, :], rhs_of(k), k == 0, k == K - 1, rreads, [rp])

    jobs = []
    for s in range(NSUB):
        t0 = s * TT
        tsl = slice(t0, t0 + TT)

        def load_acts(job, s=s, t0=t0):
            S_.dma("sp", C.hT[:], hv[:, :, t0:t0 + TT], [r_hl[s]], [C.r_hT])
            if dyn is None:
                S_.dma("sp", C.ost[:], o_sh_d[:, :, t0:t0 + TT].rearrange("h r t -> r h t"), [], [C.r_ost])
            else:
                o3 = dyn["o_all3"]
                S_.dma("sp", C.ost[:], lambda d: o3[bass.ds(d["pid"], 1), :, :, t0:t0 + TT].rearrange("a h r t -> r (a h) t"),
                       dyn["r_o_all"], [C.r_ost])
        jobs.append(Job(load_acts, lambda job: None))

        for i in range(4):
            def c_xc(job, s=s, i=i):
                pt, rp = next_bank(T)
                group(pt, rp, job.w, 0, 8, lambda k: C.hT[:, k, :], [job.rw, C.r_hT])
                S_.op("act", lambda e: e.copy(out=C.xc[:], in_=pt[:, :]), [rp], [C.r_xc])
                if s == 0:
                    p2, rp2 = next_bank(T)
                    group(p2, rp2, job.w, 0, 8, lambda k: C.hh[:, k, :], [job.rw, C.r_hh], ncol=2)
                    S_.op("act", lambda e: e.copy(out=C.xh[:], in_=p2[:, 0:2]), [rp2], [C.r_xh])
            jobs.append(Job(slab(w_in[:, 3072 + i * 128:3072 + (i + 1) * 128], 8), c_xc))

            def c_cc(job, s=s, i=i):
                pt, rp = next_bank(T)
                group(pt, rp, job.w, 0, 8, lambda k: C.hT[:, k, :], [job.rw, C.r_hT])
                if s == 0:
                    p2, rp2 = next_bank(T)
                    group(p2, rp2, job.w, 0, 8, lambda k: C.hh[:, k, :], [job.rw, C.r_hh], ncol=2)
                    S_.op("dve", lambda e: e.tensor_tensor(out=C.hu[:, i, :], in0=p2[:, 0:2], in1=C.xh[:], op=ALU.mult),
                          [rp2, C.r_xh], [C.r_hu])
                S_.op("dve", lambda e: e.tensor_copy(out=C.u[:, 0:2], in_=C.hu[:, i, :]), [C.r_hu], [C.r_u])
                S_.op("dve", lambda e: e.tensor_tensor(out=C.u[:, 2:TT + 2], in0=pt[:, :], in1=C.xc[:], op=ALU.mult),
                      [rp, C.r_xc], [C.r_u])
                S_.op("dve", lambda e: e.tensor_copy(out=C.hu[:, i, :], in_=C.u[:, TT:TT + 2]), [C.r_u], [C.r_hu])
                cw0 = l * 12 + i * 3
                S_.op("dve", lambda e: e.tensor_scalar(out=C.y[:], in0=C.u[:, 0:TT], scalar1=C.cw[:, cw0:cw0 + 1], scalar2=None,
                                                       op0=ALU.mult), [C.r_u, C.r_cw], [C.r_y])
                S_.op("dve", lambda e: e.scalar_tensor_tensor(out=C.y[:], in0=C.u[:, 1:TT + 1], scalar=C.cw[:, cw0 + 1:cw0 + 2],
                                                              in1=C.y[:], op0=ALU.mult, op1=ALU.add), [C.r_u, C.r_cw, C.r_y], [C.r_y])
                S_.op("dve", lambda e: e.scalar_tensor_tensor(out=C.y[:], in0=C.u[:, 2:TT + 2], scalar=C.cw[:, cw0 + 2:cw0 + 3],
                                                              in1=C.y[:], op0=ALU.mult, op1=ALU.add), [C.r_u, C.r_cw, C.r_y], [C.r_y])
            jobs.append(Job(slab(w_in[:, 4096 + i * 128:4096 + (i + 1) * 128], 8), c_cc))

            def c_bc(job, i=i):
                pt, rp = next_bank(T)
                group(pt, rp, job.w, 0, 8, lambda k: C.hT[:, k, :], [job.rw, C.r_hT])
                S_.op("dve", lambda e: e.tensor_tensor(out=C.ocT[:, i, :], in0=pt[:, :], in1=C.y[:], op=ALU.mult),
                      [rp, C.r_y], [C.r_ocT])
            jobs.append(Job(slab(w_in[:, 3584 + i * 128:3584 + (i + 1) * 128], 8), c_bc))

        for oc in range(8):
            for br in range(3):
                def c_gate(job, oc=oc, br=br):
                    pt, rp = next_bank(T)
                    group(pt, rp, job.w, 0, 8, lambda k: C.hT[:, k, :], [job.rw, C.r_hT])
                    bcol = l * 24 + br * 8 + oc
                    S_.op("act", lambda e: e.activation(out=C.gs[br][:], in_=pt[:, :], func=AF.Sigmoid,
                                                        bias=C.bg[:, bcol:bcol + 1], scale=1.0),
                          [rp, C.r_bg], [C.r_gs[br]])
                c0 = 4608 + br * 1024 + oc * 128
                jobs.append(Job(slab(w_in[:, c0:c0 + 128], 8), c_gate))

            def c_proj(job, oc=oc):
                for br in range(3):
                    pt, rp = next_bank(T)
                    if br < 2:
                        ps_ = slice(64 * br, 64 * br + 64)
                        for h in range(8):
                            mm(S_, pt[:, :], job.w[ps_, h, :], C.ost[ps_, h, :], h == 0, h == 7, [job.rw, C.r_ost], [rp])
                    else:
                        for k in range(4):
                            mm(S_, pt[:, :], job.w[:, 8 + k, :], C.ocT[:, k, :], k == 0, k == 3, [job.rw, C.r_ocT], [rp])
                    S_.op("dve", lambda e, br=br, pt=pt: e.tensor_tensor(out=C.t[br][:], in0=pt[:, :], in1=C.gs[br][:], op=ALU.mult),
                          [rp, C.r_gs[br]], [C.r_t[br]])
                S_.op("pool", lambda e: e.tensor_tensor(out=C.t[0][:], in0=C.t[0][:], in1=C.t[1][:], op=ALU.add),
                      [C.r_t[0], C.r_t[1]], [C.r_t[0]])
                S_.op("pool", lambda e: e.tensor_tensor(out=C.mT[:, oc, :], in0=C.t[0][:], in1=C.t[2][:], op=ALU.add),
                      [C.r_t[0], C.r_t[2]], [C.r_mT])
            jobs.append(Job(slab3([W["wpa"][l][:, oc * 128:(oc + 1) * 128], W["wps"][l][:, oc * 128:(oc + 1) * 128],
                                   W["wpc"][l][:, oc * 128:(oc + 1) * 128]]), c_proj))

        for oc in range(8):
            def c_wo(job, s=s, oc=oc, tsl=tsl):
                pt, rp = next_bank(T)
                group(pt, rp, job.w, 0, 8, lambda k: C.mT[:, k, :], [job.rw, C.r_mT])
                S_.op("dve", lambda e: e.tensor_tensor(out=T.xT[:, oc, tsl], in0=pt[:, :], in1=T.xT[:, oc, tsl], op=ALU.add),
                      [rp, T.r_x[s][oc]], [T.r_x[s][oc]])
            jobs.append(Job(slab(W["w_out"][l][:, oc * 128:(oc + 1) * 128], 8), c_wo))

        def c_norm2(job, s=s):
            emit_norm(cx, T, s, 2 * l + 1, C.hT, C.r_hT)
        jobs.append(Job(None, c_norm2))

        for fc in range(NFC):
            def c_fg(job, fc=fc):
                pt, rp = next_bank(T)
                group(pt, rp, job.w, 0, 8, lambda k: C.hT[:, k, :], [job.rw, C.r_hT])
                S_.op("act", lambda e: e.activation(out=C.sil[:], in_=pt[:, :], func=AF.Silu), [rp], [C.r_sil])
            jobs.append(Job(slab(W["wg"][l][:, fc * 128:(fc + 1) * 128], 8), c_fg))

            def c_fu(job, fc=fc):
                pt, rp = next_bank(T)
                group(pt, rp, job.w, 0, 8, lambda k: C.hT[:, k, :], [job.rw, C.r_hT])
                S_.op("dve", lambda e: e.tensor_tensor(out=C.aT[:, fc, :], in0=pt[:, :], in1=C.sil[:], op=ALU.mult),
                      [rp, C.r_sil], [C.r_aT[fc]])
            jobs.append(Job(slab(W["wu"][l][:, fc * 128:(fc + 1) * 128], 8), c_fu))

        for oc in range(8):
            def c_fd(job, s=s, oc=oc, tsl=tsl):
                pt, rp = next_bank(T)
                for k in range(NFC):
                    mm(S_, pt[:, :], job.w[:, k, :], C.aT[:, k, :], k == 0, k == NFC - 1, [job.rw, C.r_aT[k]], [rp])
                S_.op("dve", lambda e: e.tensor_tensor(out=T.xT[:, oc, tsl], in0=pt[:, :], in1=T.xT[:, oc, tsl], op=ALU.add),
                      [rp, T.r_x[s][oc]], [T.r_x[s][oc]])
            jobs.append(Job(slab(W["wd"][l][:, oc * 128:(oc + 1) * 128], NFC), c_fd))

        def c_out(job, s=s, t0=t0):
            if final:
                emit_norm(cx, T, s, 2 * DEPTH, C.fout, C.r_fout)
                S_.dma("sp", out_d.rearrange("(k p) t -> p k t", p=128)[:, :, t0:t0 + TT], C.fout[:], [C.r_fout], [])
            else:
                ho = T.hout[s % 2]
                rh = T.r_hout[s % 2]
                emit_norm(cx, T, s, 2 * (l + 1), ho, rh)
                S_.dma("sp", out_d.rearrange("(k p) t -> p k t", p=128)[:, :, t0:t0 + TT], ho[:], [rh], [r_hl[s]])
        jobs.append(Job(None, c_out))
    run_jobs(jobs, look=2)
    return CS


NG = 2 * DEPTH + 1


def declare_weights(nc):
    W = {}
    W["w_in"] = nc.dram_tensor("w_in", [DEPTH, D, INW], F32, kind="ExternalInput").ap()
    W["wpa"] = nc.dram_tensor("wpa", [DEPTH, 512, D], F32, kind="ExternalInput").ap()
    W["wps"] = nc.dram_tensor("wps", [DEPTH, 512, D], F32, kind="ExternalInput").ap()
    W["wpc"] = nc.dram_tensor("wpc", [DEPTH, 512, D], F32, kind="ExternalInput").ap()
    W["w_out"] = nc.dram_tensor("w_out", [DEPTH, D, D], F32, kind="ExternalInput").ap()
    W["wg"] = nc.dram_tensor("wg", [DEPTH, D, DFF], F32, kind="ExternalInput").ap()
    W["wu"] = nc.dram_tensor("wu", [DEPTH, D, DFF], F32, kind="ExternalInput").ap()
    W["wd"] = nc.dram_tensor("wd", [DEPTH, DFF, D], F32, kind="ExternalInput").ap()
    W["bg"] = nc.dram_tensor("bg", [128, 24 * DEPTH], F32, kind="ExternalInput").ap()
    W["cw"] = nc.dram_tensor("cw", [128, 12 * DEPTH], F32, kind="ExternalInput").ap()
    return W


def host_weights(inp):
    f = lambda a: np.ascontiguousarray(np.asarray(a, dtype=np.float32))
    bg = f(inp["b_gate"]).reshape(DEPTH, 24, 128).transpose(2, 0, 1).reshape(128, DEPTH * 24)
    cw = f(inp["conv_w"]).reshape(DEPTH, 3, 4, 128).transpose(3, 0, 2, 1).reshape(128, DEPTH * 12)
    return {"w_in": f(inp["w_in"]), "wpa": f(inp["w_proj_moba"]), "wps": f(inp["w_proj_sb"]), "wpc": f(inp["w_proj_conv"]),
            "w_out": f(inp["w_out"]), "wg": f(inp["w_ffn_gate"]), "wu": f(inp["w_ffn_up"]), "wd": f(inp["w_ffn_down"]),
            "bg": f(bg), "cw": f(cw)}


def host_gains(inp):
    G = [inp["norm_mix_g"][0], inp["norm_ffn_g"][0], inp["norm_mix_g"][1], inp["norm_ffn_g"][1], inp["norm_final_g"]]
    G = np.stack([np.asarray(g, np.float32) for g in G], 0)
    return np.ascontiguousarray(G.reshape(NG, 8, 128).transpose(2, 0, 1).reshape(128, NG * 8))


def build_A():
    nc = bass.Bass("TRN2", target_bir_lowering=False)
    xT_d = nc.dram_tensor("xT", [D, TSH], F32, kind="ExternalInput").ap()
    gains_d = nc.dram_tensor("gains", [128, NG * 8], F32, kind="ExternalInput").ap()
    cb_d = nc.dram_tensor("cb", [128, CB_W], BF16, kind="ExternalInput").ap()
    hT_o = nc.dram_tensor("hT_o", [D, TSH], BF16, kind="ExternalOutput").ap()
    with ExitStack() as stack:
        cx = Ctx(nc, stack)
        T = tok_alloc(cx)
        tok_load_x(cx, T, xT_d, gains_d, cb_d)
        phase_A(cx, T, 0, hT_o)
        cx.S.finish("sp")
        cx.S.emit(nc, stack)
    return nc


def build_C(l, final):
    nc = bass.Bass("TRN2", target_bir_lowering=False)
    xT_d = nc.dram_tensor("xT", [D, TSH], F32, kind="ExternalInput").ap()
    gains_d = nc.dram_tensor("gains", [128, NG * 8], F32, kind="ExternalInput").ap()
    cb_d = nc.dram_tensor("cb", [128, CB_W], BF16, kind="ExternalInput").ap()
    hT_d = nc.dram_tensor("hT_loc", [D, TSH], BF16, kind="ExternalInput").ap()
    hh_d = nc.dram_tensor("hh", [128, 8, 2], BF16, kind="ExternalInput").ap()
    o_sh = nc.dram_tensor("o_sh", [NCORES, 128, TSH], BF16, kind="ExternalInput").ap()
    W = declare_weights(nc)
    if final:
        out_d = nc.dram_tensor("outT", [D, TSH], F32, kind="ExternalOutput").ap()
    else:
        out_d = nc.dram_tensor("hT_o", [D, TSH], BF16, kind="ExternalOutput").ap()
        x2_d = nc.dram_tensor("x2T", [D, TSH], F32, kind="ExternalOutput").ap()
    with ExitStack() as stack:
        cx = Ctx(nc, stack)
        T = tok_alloc(cx)
        tok_load_x(cx, T, xT_d, gains_d, cb_d)
        phase_C(cx, T, W, l, hT_d, hh_d, o_sh, final, out_d)
        if not final:
            xv = x2_d.rearrange("(k p) t -> p k t", p=128)
            for k in range(8):
                cx.S.dma("sp", xv[:, k, :], T.xT[:, k, :], [T.r_x[s][k] for s in range(NSUB)], [])
        cx.S.finish("sp")
        cx.S.emit(nc, stack)
    return nc


def halo_from_hT(hT_sh):
    out = []
    for r in range(NCORES):
        if r % (NCORES // B) == 0:
            hh = np.zeros((D, 2), ml_dtypes.bfloat16)
        else:
            hh = hT_sh[r - 1][:, TSH - 2:TSH]
        out.append(np.ascontiguousarray(hh.reshape(8, 128, 2).transpose(1, 0, 2)))
    return out


def kernel_unfused(**inp):
    x = np.asarray(inp["x"], np.float32)
    cores = list(range(NCORES))
    xT_sh = [np.ascontiguousarray(x.reshape(NTOK, D)[r * TSH:(r + 1) * TSH].T) for r in cores]
    gains = host_gains(inp)
    cbn = build_cb()
    Wh = host_weights(inp)
    w_in = np.asarray(inp["w_in"], np.float32)
    res = run_bass_kernel_spmd(build_A(), [{"xT": xT_sh[r], "gains": gains, "cb": cbn} for r in cores], core_ids=cores)
    hT_sh = [res.results[r]["hT_o"] for r in cores]
    out = None
    for l in range(DEPTH):
        hT_all = np.ascontiguousarray(np.stack(hT_sh, 0))
        o_all = run_B(hT_all, w_in[l])
        halos = halo_from_hT(hT_sh)
        final = (l == DEPTH - 1)
        in_maps = []
        for r in cores:
            m = {"xT": xT_sh[r], "gains": gains, "cb": cbn, "hT_loc": hT_sh[r], "hh": halos[r],
                 "o_sh": np.ascontiguousarray(o_all[:, :, r * TSH:(r + 1) * TSH])}
            m.update(Wh)
            in_maps.append(m)
        res = run_bass_kernel_spmd(build_C(l, final), in_maps, core_ids=cores)
        if final:
            out = np.concatenate([res.results[r]["outT"].T for r in cores], axis=0)
        else:
            hT_sh = [res.results[r]["hT_o"] for r in cores]
            xT_sh = [res.results[r]["x2T"] for r in cores]
    return np.ascontiguousarray(out.reshape(B, S, D).astype(np.float32))


def build_fused():
    nc = bass.Bass("TRN2", target_bir_lowering=False)
    xT_d = nc.dram_tensor("xT", [D, TSH], F32, kind="ExternalInput").ap()
    gains_d = nc.dram_tensor("gains", [128, NG * 8], F32, kind="ExternalInput").ap()
    cb_d = nc.dram_tensor("cb", [128, CB_W], BF16, kind="ExternalInput").ap()
    cfB_d = nc.dram_tensor("cfB", [128, 128], F32, kind="ExternalInput").ap()
    kconst_d = nc.dram_tensor("kconst", [35, S], BF16, kind="ExternalInput").ap()
    qconst_d = nc.dram_tensor("qconst", [2, S], BF16, kind="ExternalInput").ap()
    hmask_d = nc.dram_tensor("hmask", [128, 1], F32, kind="ExternalInput").ap()
    wB_d = [nc.dram_tensor("wB%d" % l, [D, 384], F32, kind="ExternalInput").ap() for l in range(DEPTH)]
    W = declare_weights(nc)
    outT = nc.dram_tensor("outT", [D, TSH], F32, kind="ExternalOutput").ap()
    hT_loc = nc.dram_tensor("hT_loc_i", [D, TSH], BF16).ap()
    hT_all = nc.dram_tensor("hT_all_i", [NCORES * D, TSH], BF16).ap()
    o_loc = nc.dram_tensor("o_loc_i", [128, NTOK], BF16).ap()
    o_all = nc.dram_tensor("o_all_i", [NCORES * NCORES * 128, TSH], BF16).ap()
    x2_d = nc.dram_tensor("x2_i", [D, TSH], F32).ap()
    global HT_CHUNKED
    HT_CHUNKED = True
    hT_all3 = hT_all.rearrange("(k r p) t -> k r p t", k=8, r=NCORES)
    o_all3 = o_all.rearrange("(c r p) t -> c r p t", c=NCORES, r=NCORES)
    o_loc2 = o_loc
    o_loc = nc.dram_tensor("o_locc_i", [NCORES * 128, TSH], BF16).ap()
    o_loc3 = o_loc.rearrange("(c p) t -> c p t", c=NCORES)

    def gather_h():
        for k in range(8):
            S_.coll("AllGather", [hT_loc[k * 128:(k + 1) * 128, :]], [hT_all[k * 1024:(k + 1) * 1024, :]], r_hT_loc, [r_hT_all])

    def gather_o_shard(c):
        S_.coll("AllGather", [o_loc[c * 128:(c + 1) * 128, :]], [o_all[c * 1024:(c + 1) * 1024, :]], [r_o_loc[c]], [r_o_all[c]])
    with ExitStack() as stack:
        cx = Ctx(nc, stack)
        S_ = cx.S
        cx.get_banks()
        r_hT_loc = mkres(NSUB)
        r_hT_all, r_x2 = Res(), Res()
        r_o_loc = mkres(NCORES)
        r_o_all = mkres(NCORES)
        m0 = cx.mark()
        T = tok_alloc(cx)
        tok_load_x(cx, T, xT_d, gains_d, cb_d)
        phase_A(cx, T, 0, hT_loc, r_hT_loc)
        gather_h()
        S_.barrier()
        cx.release(m0)
        for l in range(DEPTH):
            final = (l == DEPTH - 1)
            phase_B(cx, hT_all3, wB_d[l], cb_d, cfB_d, kconst_d, qconst_d, o_loc3, r_hT_all, r_o_loc, do_finish=False,
                    shard_done=gather_o_shard)
            S_.barrier()
            cx.release(m0)
            T = tok_alloc(cx)
            tok_load_x(cx, T, xT_d if l == 0 else x2_d, gains_d, cb_d, None if l == 0 else r_x2)
            dyn = {"r_hT_loc": r_hT_loc, "hT_all3": hT_all3, "r_hT_all": r_hT_all, "hmask": hmask_d, "o_all3": o_all3, "r_o_all": r_o_all}
            phase_C(cx, T, W, l, hT_loc, None, None, final, outT if final else hT_loc, dyn=dyn)
            if not final:
                xv = x2_d.rearrange("(k p) t -> p k t", p=128)
                for k in range(8):
                    S_.dma("sp", xv[:, k, :], T.xT[:, k, :], [T.r_x[s][k] for s in range(NSUB)], [r_x2])
                gather_h()
            S_.barrier()
            cx.release(m0)
        S_.finish("sp")
        print("SBUF peak", cx.peak)
        S_.emit(nc, stack)
    return nc


def kernel_fused(**inp):
    x = np.asarray(inp["x"], np.float32)
    cores = list(range(NCORES))
    gains = host_gains(inp)
    cbn = build_cb()
    Wh = host_weights(inp)
    w_in = np.asarray(inp["w_in"], np.float32)
    sl = slopes()
    in_maps = []
    for r in cores:
        kc, qc = build_kq_const(sl[r])
        m = {"xT": np.ascontiguousarray(x.reshape(NTOK, D)[r * TSH:(r + 1) * TSH].T), "gains": gains, "cb": cbn,
             "cfB": build_cfB(sl[r]), "kconst": kc, "qconst": qc,
             "hmask": np.full((128, 1), 0.0 if r % (NCORES // B) == 0 else 1.0, np.float32)}
        for l in range(DEPTH):
            m["wB%d" % l] = wB_for_head(w_in[l], r)
        m.update(Wh)
        in_maps.append(m)
    res = run_bass_kernel_spmd(build_fused(), in_maps, core_ids=cores)
    out = np.concatenate([res.results[r]["outT"].T for r in cores], axis=0)
    return np.ascontiguousarray(out.reshape(B, S, D).astype(np.float32))


def kernel(**inp):
    return kernel_fused(**inp)
```

```python
import numpy as np
import ml_dtypes
from contextlib import ExitStack
import concourse.bass as bass
import concourse.mybir as mybir
from concourse.bass_utils import run_bass_kernel_spmd

F32 = mybir.dt.float32
BF16 = mybir.dt.bfloat16
AF = mybir.ActivationFunctionType
ALU = mybir.AluOpType
AX = mybir.AxisListType

NCORES = 8
D = 1024
B = 2
S = 8192
NTOK = B * S
TSH = NTOK // NCORES
DEPTH = 2
DFF = 2816
NFC = DFF // 128
INW = 7680
EPS = 1e-6
SCALE = 0.125
BIG = 16384.0
TT = 512
NSUB = TSH // TT
NQT = S // TT
NKC = S // 128
NBLK = S // 256
DBG_STOP = 0
HT_CHUNKED = False


class Res:
    __slots__ = ("w", "r", "excl")

    def __init__(self, excl=False):
        self.w = None
        self.r = {}
        self.excl = excl


def mkres(n, excl=False):
    return [Res(excl) for _ in range(n)]


class Sched:
    ENGS = ("pe", "act", "dve", "pool", "sp")
    QSLOTS = {"sp": list(range(0, 8)), "pool": list(range(8, 14)), "act": list(range(14, 16))}

    def __init__(self):
        self.q = {e: [] for e in self.ENGS}
        self.n = {e: 0 for e in self.ENGS}
        self.flag = {e: set() for e in self.ENGS}
        self.waited = {e: {} for e in self.ENGS}
        self.slot_n = [0] * 16
        self.slot_rr = {k: 0 for k in self.QSLOTS}
        self.n_cc = 0
        self.need_pid = set()

    def _need(self, eng, reads, writes):
        need = {}
        for r in reads:
            if r.w is not None:
                k, v = r.w
                if k == eng and eng == "pe":
                    continue
                if v > need.get(k, 0):
                    need[k] = v
            if r.excl:
                for k, v in r.r.items():
                    if k != eng and v > need.get(k, 0):
                        need[k] = v
        for w in writes:
            if w.w is not None:
                k, v = w.w
                if k != eng and v > need.get(k, 0):
                    need[k] = v
            for k, v in w.r.items():
                if k != eng and v > need.get(k, 0):
                    need[k] = v
        return need

    def _waits(self, eng, need):
        wd = self.waited[eng]
        for k, v in need.items():
            if wd.get(k, 0) >= v:
                continue
            wd[k] = v
            if k[0] != "q" and k != "cc":
                self.flag[k].add(v)
            self.q[eng].append(("wait", k, v))

    def op(self, eng, fn, reads=(), writes=()):
        self._waits(eng, self._need(eng, reads, writes))
        self.n[eng] += 1
        idx = self.n[eng]
        self.q[eng].append(("op", fn, idx))
        for r in reads:
            r.r[eng] = idx
        for w in writes:
            w.w = (eng, idx)
            w.r = {}
        return idx

    def dma(self, queue, out, in_, reads=(), writes=()):
        sl = self.QSLOTS[queue]
        slot = sl[self.slot_rr[queue] % len(sl)]
        self.slot_rr[queue] += 1
        key = "q%d" % slot
        need = self._need(None, reads, writes)
        if self.slot_n[slot] > 0:
            need[key] = max(need.get(key, 0), self.slot_n[slot])
        self._waits(queue, need)
        self.slot_n[slot] += 1
        v = self.slot_n[slot]
        self.q[queue].append(("dma", out, in_, slot))
        if callable(out) or callable(in_):
            self.need_pid.add(queue)
        for r in reads:
            r.r[key] = v
        for w in writes:
            w.w = (key, v)
            w.r = {}

    def finish(self, queue):
        need = {}
        for s in range(16):
            if self.slot_n[s] > 0:
                need["q%d" % s] = self.slot_n[s]
        self._waits(queue, need)

    def barrier(self):
        for e in self.ENGS:
            need = {}
            for k in ("pe", "act", "dve", "pool"):
                if k != e and self.n[k] > 0:
                    need[k] = self.n[k]
            for s in range(16):
                if self.slot_n[s] > 0:
                    need["q%d" % s] = self.slot_n[s]
            if self.n_cc > 0:
                need["cc"] = self.n_cc
            self._waits(e, need)

    def coll(self, kind, ins, outs, reads=(), writes=()):
        need = self._need(None, reads, writes)
        self._waits("pool", need)
        self.n_cc += 1
        self.q["pool"].append(("coll", kind, ins, outs))
        for r in reads:
            r.r["cc"] = self.n_cc
        for w in writes:
            w.w = ("cc", self.n_cc)
            w.r = {}

    def emit(self, nc, stack):
        sems = {}
        for k in ("pe", "act", "dve", "pool"):
            sems[k] = stack.enter_context(nc.semaphore("s_" + k))
        for s in range(16):
            sems["q%d" % s] = stack.enter_context(nc.semaphore("s_q%d" % s))
        sems["cc"] = stack.enter_context(nc.semaphore("s_cc"))
        rank = {}
        for k in self.ENGS:
            fl = sorted(self.flag[k])
            rank[k] = {idx: i + 1 for i, idx in enumerate(fl)}
        block = stack.enter_context(nc.Block())
        q = self.q

        need_pid = self.need_pid

        def run(ek, e):
            rk = rank[ek]
            dyn = {}
            if ek in need_pid:
                pid = e.partition_id()
                dyn["pid"] = pid
                dyn["prev"] = (pid + (NCORES - 1)) % NCORES
            for item in q[ek]:
                t = item[0]
                if t == "wait":
                    _, k, v = item
                    val = 16 * v if k[0] == "q" else (v if k == "cc" else rank[k][v])
                    e.wait_ge(sems[k], val)
                elif t == "op":
                    inst = item[1](e)
                    if item[2] in rk:
                        inst.then_inc(sems[ek], 1)
                elif t == "coll":
                    _, kind, ins, outs = item
                    e.collective_compute(kind, ALU.bypass, replica_groups=[list(range(NCORES))], ins=ins, outs=outs).then_inc(sems["cc"])
                else:
                    _, out, in_, slot = item
                    if callable(in_):
                        in_ = in_(dyn)
                    if callable(out):
                        out = out(dyn)
                    e.dma_start(out=out, in_=in_).then_inc(sems["q%d" % slot], 16)

        @block.tensor
        def _(e):
            run("pe", e)

        @block.scalar
        def _(e):
            run("act", e)

        @block.vector
        def _(e):
            run("dve", e)

        @block.gpsimd
        def _(e):
            run("pool", e)

        @block.sync
        def _(e):
            run("sp", e)


class Ctx:
    SB_BASE = 16512
    SB_END = 229376

    def __init__(self, nc, stack):
        self.nc = nc
        self.stack = stack
        self.S = Sched()
        self._n = 0
        self.ptr = self.SB_BASE
        self.peak = self.ptr
        self.banks = None

    def sb(self, shape, dt, name=None):
        self._n += 1
        nbytes = int(np.prod(shape[1:])) * (2 if dt == BF16 else 4)
        nbytes = (nbytes + 31) // 32 * 32
        off = self.ptr
        self.ptr += nbytes
        self.peak = max(self.peak, self.ptr)
        assert self.ptr <= self.SB_END, "SBUF overflow %d" % self.ptr
        return self.nc.alloc_sbuf_tensor_at("%s_%d" % (name or "sb", self._n), list(shape), dt, offset=off)

    def mark(self):
        return self.ptr

    def release(self, m):
        self.ptr = m

    def ps(self, name=None):
        self._n += 1
        return self.stack.enter_context(self.nc.psum_tensor("%s_%d" % (name or "ps", self._n), [128, 512], F32))

    def get_banks(self):
        if self.banks is None:
            self.banks = [self.ps("bank") for _ in range(8)]
        return self.banks


def mm(S, out, lhsT, rhs, start, stop, reads, writes):
    S.op("pe", lambda e: e.matmul(out, lhsT, rhs, start=start, stop=stop, skip_group_check=True), reads, writes)


CB_ONES = 0
CB_IDENT = 128
CB_NTRI = 256
CB_NLOW = 384
CB_MLE = 512
CB_MLT = 512 + 2048
CB_W = 512 + 4096


def build_cb():
    cb = np.zeros((128, CB_W), np.float32)
    cb[:, CB_ONES:CB_ONES + 128] = 1.0
    cb[:, CB_IDENT:CB_IDENT + 128] = np.eye(128)
    jj = np.arange(128)[:, None]
    ss = np.arange(128)[None, :]
    cb[:, CB_NTRI:CB_NTRI + 128] = -1.0 * (jj >= ss)
    cb[:, CB_NLOW:CB_NLOW + 128] = -1.0 * (jj < ss)
    t = np.arange(512)[None, :]
    for j in range(4):
        cb[:, CB_MLE + 512 * j:CB_MLE + 512 * (j + 1)] = (128 * j + jj <= t)
        cb[:, CB_MLT + 512 * j:CB_MLT + 512 * (j + 1)] = (128 * j + jj < t)
    return cb.astype(ml_dtypes.bfloat16)


def build_cfB(slope):
    cf = np.zeros((128, 128), np.float32)
    j = np.arange(64)
    m = np.where(j < 32, 0.0, np.where(j == 32, BIG, -BIG))
    ab = np.where(j <= 32, BIG - slope * 256.0 * (32 - j), 0.0)
    cf[:, 0:64] = m[None, :]
    cf[:, 64:128] = ab[None, :]
    return cf


def build_kq_const(slope):
    s = np.arange(S)
    kc = np.zeros((35, S), np.float32)
    kc[s // 256, s] = 1.0
    kc[32, :] = 1.0
    kc[33, :] = slope * (s % 256)
    kc[34, :] = 1.0
    qc = np.zeros((2, S), np.float32)
    qc[0, :] = 1.0
    qc[1, :] = -slope * (s % 256)
    return kc.astype(ml_dtypes.bfloat16), qc.astype(ml_dtypes.bfloat16)


def phase_B(cx, hT_all, wB, cb_d, cfB_d, kconst_d, qconst_d, o_loc, r_hT_all=None, r_o_loc=None, do_finish=True, shard_done=None):
    S_ = cx.S
    cb = cx.sb([128, CB_W], BF16, "cb")
    cf = cx.sb([128, 128], F32, "cfB")
    onesf = cx.sb([128, 64], F32, "onesf")
    wq = cx.sb([128, 8, 384], BF16, "wq")
    Qa = cx.sb([128, S], BF16, "Qa")
    Ka = cx.sb([128, S], BF16, "Ka")
    Va = cx.sb([128, NKC, 65], BF16, "Va")
    Qs = cx.sb([128, S], BF16, "Qs")
    Ks = cx.sb([128, S], BF16, "Ks")
    Vs = cx.sb([128, NKC, 64], BF16, "Vs")
    hbuf = [cx.sb([128, 8, TT], BF16, "hb") for _ in range(2)]
    kmean = cx.sb([128, 32], F32, "kmean")
    kmean_b = cx.sb([128, 32], BF16, "kmeanb")
    kmaxt = cx.sb([128, NQT], F32, "kmaxt")
    negc = cx.sb([128, 1], F32, "negc")
    sq = cx.sb([128, TT], BF16, "sq")
    gate = cx.sb([128, 32], F32, "gate")
    top8 = cx.sb([128, 8], F32, "top8")
    thr = cx.sb([128, 1], F32, "thr")
    sel = cx.sb([128, 32], F32, "sel")
    mpad = [cx.sb([128, 96], BF16, "mpad") for _ in range(2)]
    gate4 = [cx.sb([128, 4, 32], F32, "gate4") for _ in range(2)]
    top84 = [cx.sb([128, 4, 8], F32, "top84") for _ in range(2)]
    thr4 = [cx.sb([128, 4, 1], F32, "thr4") for _ in range(2)]
    sel4 = [cx.sb([128, 4, 32], F32, "sel4") for _ in range(2)]
    mpad4 = [cx.sb([128, 4, 96], BF16, "mpad4") for _ in range(2)]
    r_gate4, r_top84, r_thr4, r_sel4, r_mpad4 = mkres(2), mkres(2), mkres(2), mkres(2), mkres(2)
    nqb = [cx.sb([128, TT], BF16, "nq") for _ in range(2)]
    Ebuf = [cx.sb([128, TT], F32, "E") for _ in range(4)]
    Xbuf = [cx.sb([128, TT], F32, "X") for _ in range(2)]
    r_X = mkres(2)
    SPb = [cx.sb([128, TT], BF16, "SP") for _ in range(3)]
    Wb = [cx.sb([128, TT], BF16, "W") for _ in range(2)]
    Pb = [cx.sb([128, TT], BF16, "P") for _ in range(2)]
    rl = cx.sb([128, TT], F32, "rl")
    oaf = cx.sb([128, TT], F32, "oaf")
    oout = [cx.sb([128, TT], BF16, "oout") for _ in range(2)]
    bk = cx.get_banks()
    pz = bk[0:2]
    psm = bk[2:4]
    pR, pOs, pOa, pX = bk[4], bk[5], bk[6], bk[7]
    r_hT_all = r_hT_all if r_hT_all is not None else Res()
    r_o_loc = r_o_loc if r_o_loc is not None else Res()

    def r_o_of(g):
        return r_o_loc[g // TSH] if isinstance(r_o_loc, list) else r_o_loc

    r_cb, r_cf, r_onesf, r_wq = Res(), Res(), Res(), Res()
    r_Qa = mkres(NQT)
    r_Qm = mkres(NQT)
    r_Qst = mkres(NQT)
    r_Qc = Res()
    r_Ka = mkres(NQT)
    r_Kc = Res()
    r_Va = mkres(NQT)
    r_Vone = Res()
    r_Qs = mkres(NQT)
    r_Ks = mkres(NQT)
    r_Vs = mkres(NQT)
    r_hb = mkres(2)
    r_kmean, r_kmeanb, r_kmaxt, r_negc, r_sq = Res(), Res(), Res(), Res(), Res()
    r_gate, r_top8, r_thr, r_sel = Res(), Res(), Res(), Res()
    r_mpad = mkres(2)
    r_nq = mkres(2)
    r_E = mkres(4)
    r_SP = mkres(3)
    r_W = mkres(2)
    r_P = mkres(2)
    r_rl, r_oaf = Res(), Res()
    r_oout = mkres(2)
    r_pz = mkres(2, True)
    r_psm = mkres(2, True)
    r_pR, r_pOs, r_pOa, r_pX = Res(True), Res(True), Res(True), Res(True)

    S_.dma("sp", cb[:], cb_d, [], [r_cb])
    S_.dma("sp", cf[:], cfB_d, [], [r_cf])
    S_.dma("pool", wq[:], wB.rearrange("(k p) n -> p k n", p=128), [], [r_wq])
    S_.op("pool", lambda e: e.memset(onesf[:], 1.0), [], [r_onesf])
    S_.op("pool", lambda e: e.memset(kmean[:], 0.0), [], [r_kmean])
    S_.op("pool", lambda e: e.memset(Va[:, :, 64:65], 1.0), [], [r_Vone])
    for i in range(2):
        S_.op("pool", lambda e, i=i: e.memset(mpad[i][:], 0.0), [], [r_mpad[i]])
        S_.op("pool", lambda e, i=i: e.memset(mpad4[i][:], 0.0), [], [r_mpad4[i]])

    if DBG_STOP == 10:
        S_.dma("sp", o_loc[:, 0:384], wq[:, 0, :], [r_wq, r_cb, r_cf, r_onesf, r_Vone, r_kmean] + r_mpad, [])
        S_.finish("sp")
        return
    ones_bf = cb[:, CB_ONES:CB_ONES + 128]
    ident = cb[:, CB_IDENT:CB_IDENT + 128]
    ntri = cb[:, CB_NTRI:CB_NTRI + 128]
    nlow = cb[:, CB_NLOW:CB_NLOW + 128]

    for b in range(B):
        if b == 0:
            S_.dma("sp", Ka[64:99, :], kconst_d, [], [r_Kc])
            S_.dma("sp", Qa[97:99, :], qconst_d, [], [r_Qc])
        for j in range(NQT):
            hb = hbuf[j % 2]
            rh = r_hb[j % 2]
            rank, tl = divmod(b * S + j * TT, TSH)
            if HT_CHUNKED:
                hsrc = hT_all[:, rank, :, tl:tl + TT].rearrange("k p t -> p k t")
            else:
                hsrc = hT_all[rank, :, tl:tl + TT].rearrange("(k p) t -> p k t", p=128)
            S_.dma("sp", hb[:], hsrc, [r_hT_all], [rh])
            tsl = slice(j * TT, (j + 1) * TT)
            tgt = [(pz[0], r_pz[0], 0), (pz[1], r_pz[1], 128)]
            for (pt, rp, c0) in tgt:
                for k in range(8):
                    mm(S_, pt[:, :], wq[:, k, c0:c0 + 128], hb[:, k, :], k == 0, k == 7, [r_wq, rh], [rp])
            for c in range(4):
                for k in range(8):
                    mm(S_, pX[:, c * 128:(c + 1) * 128], hb[:, k, c * 128:(c + 1) * 128], wq[:, k, 256:384],
                       k == 0, k == 7, [r_wq, rh], [r_pX])
            if DBG_STOP == 11:
                S_.op("dve", lambda e: e.tensor_copy(out=Ka[0:64, 0:512], in_=pz[1][0:64, :]), [r_pz[1]], [r_Ka[j]])
                S_.op("dve", lambda e: e.tensor_copy(out=Qa[0:64, 0:512], in_=pX[0:64, :]), [r_pX, r_pz[0], r_psm[0], r_psm[1]], [r_Qa[j]])
                S_.dma("sp", o_loc[0:64, 0:512], Ka[0:64, 0:512], [r_Ka[j], r_Qa[j]], [])
                S_.finish("sp")
                return
            S_.op("act", lambda e, tsl=tsl: e.mul(out=Qa[0:64, tsl], in_=pz[0][0:64, :], mul=SCALE),
                  [r_pz[0]], [r_Qa[j]])
            S_.op("dve", lambda e, tsl=tsl: e.tensor_copy(out=Ka[0:64, tsl], in_=pz[1][0:64, :]), [r_pz[1]], [r_Ka[j]])
            S_.op("dve", lambda e, j=j: e.tensor_reduce(out=kmean[0:64, 2 * j:2 * j + 2],
                                                        in_=pz[1][0:64, :].rearrange("p (a b) -> p a b", b=256),
                                                        axis=AX.X, op=ALU.add), [r_pz[1]], [r_kmean])
            S_.op("act", lambda e, tsl=tsl: e.mul(out=Qs[64:128, tsl], in_=pz[0][64:128, :], mul=SCALE),
                  [r_pz[0]], [r_Qs[j]])
            S_.op("dve", lambda e, tsl=tsl: e.tensor_copy(out=Ks[64:128, tsl], in_=pz[1][64:128, :]), [r_pz[1]], [r_Ks[j]])
            pXv = pX[:].rearrange("p (c n) -> p c n", n=128)
            S_.op("act", lambda e, j=j, pXv=pXv: e.copy(out=Va[:, 4 * j:4 * j + 4, 0:64], in_=pXv[:, :, 0:64]),
                  [r_pX], [r_Va[j]])
            S_.op("dve", lambda e, j=j, pXv=pXv: e.tensor_copy(out=Vs[:, 4 * j:4 * j + 4, :], in_=pXv[:, :, 64:128]),
                  [r_pX], [r_Vs[j]])
            if DBG_STOP == 12:
                S_.dma("sp", o_loc[0:64, 0:512], Ka[0:64, 0:512], [r_Ka[j], r_Qa[j], r_Qs[j], r_Ks[j], r_Va[j], r_Vs[j], r_kmean], [])
                S_.finish("sp")
                return
            S_.op("pool", lambda e, tsl=tsl: e.tensor_tensor(out=sq[0:64, :], in0=Ka[0:64, tsl], in1=Ka[0:64, tsl], op=ALU.mult),
                  [r_Ka[j]], [r_sq])
            mm(S_, pR[0:97, :], ones_bf[0:64, 0:97], sq[0:64, :], True, True, [r_cb, r_sq], [r_pR])
            S_.op("dve", lambda e, j=j: e.tensor_reduce(out=kmaxt[0:97, j:j + 1], in_=pR[0:97, :], axis=AX.X, op=ALU.max),
                  [r_pR], [r_kmaxt])
        if DBG_STOP == 13:
            S_.dma("sp", o_loc[0:64, 0:512], Ka[0:64, 0:512], [r_Ka[0], r_kmaxt], [])
            S_.finish("sp")
            return
        S_.op("dve", lambda e: e.tensor_reduce(out=negc[0:97, :], in_=kmaxt[0:97, :], axis=AX.X, op=ALU.max), [r_kmaxt], [r_negc])
        S_.op("dve", lambda e: e.tensor_scalar(out=negc[0:97, :], in0=negc[0:97, :], scalar1=-0.5 * SCALE, scalar2=None, op0=ALU.mult),
              [r_negc], [r_negc])
        S_.op("dve", lambda e: e.tensor_copy(out=kmean_b[0:64, :], in_=kmean[0:64, :]), [r_kmean], [r_kmeanb])
        if DBG_STOP == 1:
            S_.dma("sp", o_loc[0:64, 0:S], Qa[0:64, :], r_Qa, [])
            S_.dma("sp", o_loc[64:128, 0:S], Ks[0:64, :], r_Ks, [])
            break
        def gate_tile(jt):
            u = jt % 2
            g4, t4, th4, s4, mp4 = gate4[u], top84[u], thr4[u], sel4[u], mpad4[u]
            rg, rt, rth, rs, rmp = r_gate4[u], r_top84[u], r_thr4[u], r_sel4[u], r_mpad4[u]
            pg, rpg = (pX, r_pX) if u == 0 else (pOs, r_pOs)
            pt_, rpt = psm[u], r_psm[u]
            for g in range(4):
                csl = slice((4 * jt + g) * 128, (4 * jt + g + 1) * 128)
                mm(S_, pg[:, 32 * g:32 * g + 32], Qa[0:64, csl], kmean_b[0:64, :], True, True, [r_Qa[jt], r_kmeanb], [rpg])
            for g in range(4):
                blk = (4 * jt + g) // 2
                S_.op("dve", lambda e, g=g, blk=blk: e.tensor_tensor(out=g4[:, g, :], in0=pg[:, 32 * g:32 * g + 32],
                                                                     in1=cf[:, 32 - blk:64 - blk], op=ALU.add), [rpg, r_cf], [rg])
            for g in range(4):
                S_.op("dve", lambda e, g=g: e.max(out=t4[:, g, :], in_=g4[:, g, :]), [rg], [rt])
            S_.op("dve", lambda e: e.tensor_scalar(out=th4[:, :, :], in0=t4[:, :, 3:4], scalar1=-BIG / 2, scalar2=None, op0=ALU.max),
                  [rt], [rth])
            for g in range(4):
                S_.op("dve", lambda e, g=g: e.tensor_scalar(out=s4[:, g, :], in0=g4[:, g, :], scalar1=th4[:, g, 0:1], scalar2=None,
                                                            op0=ALU.is_ge), [rg, rth], [rs])
            for g in range(4):
                blk = (4 * jt + g) // 2
                S_.op("dve", lambda e, g=g, blk=blk: e.tensor_tensor(out=s4[:, g, :], in0=s4[:, g, :],
                                                                     in1=cf[:, 64 + 32 - blk:64 + 64 - blk], op=ALU.mult), [rs, r_cf], [rs])
            S_.op("dve", lambda e: e.tensor_scalar(out=mp4[:, :, 64:96], in0=s4[:, :, :], scalar1=-BIG, scalar2=None, op0=ALU.add),
                  [rs], [rmp])
            for g in range(4):
                mm(S_, pt_[0:96, 128 * g:128 * g + 128], mp4[:, g, 0:96], ident, True, True, [rmp, r_cb], [rpt])
            tsl_ = slice(jt * TT, (jt + 1) * TT)
            S_.op("act", lambda e, tsl_=tsl_, pt_=pt_: e.copy(out=Qa[64:96, tsl_], in_=pt_[64:96, :]), [rpt], [r_Qm[jt]])

        for jt in range(NQT):
            gate_tile(jt)
        if DBG_STOP == 2:
            S_.dma("sp", o_loc[0:96, 0:S], Qa[0:96, :], r_Qa + r_Qm, [])
            break
        pairs = []
        for qt in range(NQT):
            n = 4 * qt + 4
            for i in range(n):
                pairs.append((qt, i, n))
        NP = len(pairs)

        def cols_of(kc, qt):
            j = kc - 4 * qt
            return (128 * j if j >= 0 else 0), j

        def sb_kc(p):
            qt, i, n = pairs[p]
            return n - 1 - i

        def mo_kc(p):
            qt, i, n = pairs[p]
            return i

        def tile_init(qt):
            tsl = slice(qt * TT, (qt + 1) * TT)
            S_.op("dve", lambda e: e.tensor_tensor(out=sq[0:64, :], in0=Qa[0:64, tsl], in1=Qa[0:64, tsl], op=ALU.mult),
                  [r_Qa[qt]], [r_sq])
            mm(S_, pX[0:97, :], ones_bf[0:64, 0:97], sq[0:64, :], True, True, [r_cb, r_sq], [r_pX])
            S_.op("dve", lambda e: e.tensor_scalar(out=Qa[96:97, tsl], in0=pX[96:97, :], scalar1=-0.5 / SCALE,
                                                   scalar2=negc[96:97, 0:1], op0=ALU.mult, op1=ALU.add),
                  [r_pX, r_negc], [r_Qst[qt]])


        def o_dst(qt, lo):
            g = b * S + qt * TT
            if HT_CHUNKED:
                c, off = divmod(g, TSH)
                return o_loc[c, lo:lo + 64, off:off + TT]
            return o_loc[lo:lo + 64, g:g + TT]

        def tile_fin_sb(qt):
            ob = oout[1]
            S_.op("dve", lambda e: e.tensor_copy(out=ob[0:64, :], in_=pOs[0:64, :]), [r_pOs], [r_oout[1]])
            g = b * S + qt * TT
            S_.dma("sp", o_dst(qt, 64), ob[0:64, :], [r_oout[1]], [r_o_of(g)])
            if shard_done is not None and (g + TT) % TSH == 0:
                shard_done(g // TSH)

        def tile_fin_mo(qt):
            ob = oout[0]
            S_.op("dve", lambda e: e.reciprocal(out=rl[64:65, :], in_=pOa[64:65, :]), [r_pOa], [r_rl])
            S_.op("act", lambda e: e.copy(out=oaf[0:64, :], in_=pOa[0:64, :]), [r_pOa], [r_oaf])
            mm(S_, pX[0:64, :], onesf[64:65, 0:64], rl[64:65, :], True, True, [r_onesf, r_rl], [r_pX])
            S_.op("dve", lambda e: e.tensor_tensor(out=ob[0:64, :], in0=oaf[0:64, :], in1=pX[0:64, :], op=ALU.mult),
                  [r_oaf, r_pX], [r_oout[0]])
            S_.dma("sp", o_dst(qt, 0), ob[0:64, :], [r_oout[0]], [r_o_of(b * S + qt * TT)])

        def f_Z(p):
            qt, i, n = pairs[p]
            if i == 0:
                tile_init(qt)
            kc = sb_kc(p)
            c0, j = cols_of(kc, qt)
            tq = slice(qt * TT + c0, (qt + 1) * TT)
            mm(S_, pz[p % 2][:, c0:TT], Ks[64:128, kc * 128:(kc + 1) * 128], Qs[64:128, tq], True, True,
               [r_Ks[kc // 4], r_Qs[qt]], [r_pz[p % 2]])

        def f_E(p):
            qt, i, n = pairs[p]
            kc = sb_kc(p)
            c0, j = cols_of(kc, qt)
            E = Ebuf[p % 4]
            S_.op("act", lambda e: e.activation(out=E[:, c0:TT], in_=pz[p % 2][:, c0:TT], func=AF.Exp), [r_pz[p % 2]], [r_E[p % 4]])

        def f_SP(p):
            qt, i, n = pairs[p]
            kc = sb_kc(p)
            c0, j = cols_of(kc, qt)
            E = Ebuf[p % 4]
            SP = SPb[p % 3]
            S_.op("act", lambda e: e.activation(out=SP[:, c0:TT], in_=E[:, c0:TT], func=AF.Ln, bias=1.0), [r_E[p % 4]], [r_SP[p % 3]])
            if j >= 0:
                mk = cb[:, CB_MLT + 512 * j + c0:CB_MLT + 512 * (j + 1)]
                S_.op("pool", lambda e: e.tensor_tensor(out=SP[:, c0:TT], in0=SP[:, c0:TT], in1=mk, op=ALU.mult),
                      [r_SP[p % 3], r_cb], [r_SP[p % 3]])

        def f_R(p):
            qt, i, n = pairs[p]
            kc = sb_kc(p)
            c0, j = cols_of(kc, qt)
            SP = SPb[p % 3]
            mm(S_, pR[:, c0:TT], ntri, SP[:, c0:TT], i == 0, True, [r_cb, r_SP[p % 3]], [r_pR])

        def f_W(p):
            qt, i, n = pairs[p]
            kc = sb_kc(p)
            c0, j = cols_of(kc, qt)
            W = Wb[p % 2]
            X = Xbuf[p % 2]
            E = Ebuf[p % 4]
            S_.op("act", lambda e: e.activation(out=X[:, c0:TT], in_=pR[:, c0:TT], func=AF.Exp), [r_pR], [r_X[p % 2]])
            S_.op("dve", lambda e: e.tensor_tensor(out=W[:, c0:TT], in0=E[:, c0:TT], in1=X[:, c0:TT], op=ALU.mult),
                  [r_E[p % 4], r_X[p % 2]], [r_W[p % 2]])
            if j >= 0:
                mk = cb[:, CB_MLT + 512 * j + c0:CB_MLT + 512 * (j + 1)]
                S_.op("pool", lambda e: e.tensor_tensor(out=W[:, c0:TT], in0=W[:, c0:TT], in1=mk, op=ALU.mult),
                      [r_W[p % 2], r_cb], [r_W[p % 2]])

        def f_corr(p):
            qt, i, n = pairs[p]
            kc = sb_kc(p)
            c0, j = cols_of(kc, qt)
            SP = SPb[p % 3]
            W = Wb[p % 2]
            if i < n - 1:
                mm(S_, pR[:, c0:TT], nlow, SP[:, c0:TT], False, True, [r_cb, r_SP[p % 3]], [r_pR])
            mm(S_, pOs[0:64, c0:TT], Vs[:, kc, :], W[:, c0:TT], i == 0, i == n - 1, [r_Vs[kc // 4], r_W[p % 2]], [r_pOs])
            if i == n - 1:
                tile_fin_sb(qt)

        def f_Sm(p):
            qt, i, n = pairs[p]
            kc = mo_kc(p)
            c0, j = cols_of(kc, qt)
            tq = slice(qt * TT + c0, (qt + 1) * TT)
            mm(S_, psm[p % 2][:, c0:TT], Ka[0:99, kc * 128:(kc + 1) * 128], Qa[0:99, tq], True, True,
               [r_Ka[kc // 4], r_Kc, r_Qa[qt], r_Qm[qt], r_Qst[qt], r_Qc], [r_psm[p % 2]])

        def f_P(p):
            qt, i, n = pairs[p]
            kc = mo_kc(p)
            c0, j = cols_of(kc, qt)
            P = Pb[p % 2]
            S_.op("act", lambda e: e.activation(out=P[:, c0:TT], in_=psm[p % 2][:, c0:TT], func=AF.Exp), [r_psm[p % 2]], [r_P[p % 2]])
            if j >= 0:
                mk = cb[:, CB_MLE + 512 * j + c0:CB_MLE + 512 * (j + 1)]
                S_.op("pool", lambda e: e.tensor_tensor(out=P[:, c0:TT], in0=P[:, c0:TT], in1=mk, op=ALU.mult),
                      [r_P[p % 2], r_cb], [r_P[p % 2]])

        def f_PVm(p):
            qt, i, n = pairs[p]
            kc = mo_kc(p)
            c0, j = cols_of(kc, qt)
            P = Pb[p % 2]
            mm(S_, pOa[0:65, c0:TT], Va[:, kc, 0:65], P[:, c0:TT], i == 0, i == n - 1,
               [r_Va[kc // 4], r_Vone, r_P[p % 2]], [r_pOa])
            if i == n - 1:
                tile_fin_mo(qt)

        def rng(p):
            return 0 <= p < NP

        for it in range(-3, NP):
            if rng(it):
                f_W(it)
            if rng(it):
                f_corr(it)
            if rng(it + 1):
                f_R(it + 1)
            if rng(it + 3):
                f_Z(it + 3)
            if rng(it + 2):
                f_E(it + 2)
            if rng(it + 2):
                f_Sm(it + 2)
            if rng(it + 1):
                f_P(it + 1)
            if rng(it + 2):
                f_SP(it + 2)
            if rng(it + 1):
                f_PVm(it + 1)
    if do_finish:
        S_.finish("sp")


def build_B():
    nc = bass.Bass("TRN2", target_bir_lowering=False)
    hT_all = nc.dram_tensor("hT_all", [NCORES, D, TSH], BF16, kind="ExternalInput").ap()
    wB = nc.dram_tensor("wB", [D, 384], F32, kind="ExternalInput").ap()
    cb_d = nc.dram_tensor("cb", [128, CB_W], BF16, kind="ExternalInput").ap()
    cfB_d = nc.dram_tensor("cfB", [128, 128], F32, kind="ExternalInput").ap()
    kconst_d = nc.dram_tensor("kconst", [35, S], BF16, kind="ExternalInput").ap()
    qconst_d = nc.dram_tensor("qconst", [2, S], BF16, kind="ExternalInput").ap()
    o_loc = nc.dram_tensor("o_loc", [128, NTOK], BF16, kind="ExternalOutput").ap()
    with ExitStack() as stack:
        cx = Ctx(nc, stack)
        phase_B(cx, hT_all, wB, cb_d, cfB_d, kconst_d, qconst_d, o_loc)
        cx.S.emit(nc, stack)
    return nc


def slopes():
    return [2.0 ** (-(h + 1)) for h in range(8)]


def wB_for_head(w_in_l, h):
    cols = []
    for base in (0, 1536, 512, 2048, 1024, 2560):
        cols.append(w_in_l[:, base + 64 * h: base + 64 * (h + 1)])
    return np.ascontiguousarray(np.concatenate(cols, axis=1))


def run_B(hT_all, w_in_l):
    nc = build_B()
    cbn = build_cb()
    sl = slopes()
    in_maps = []
    for h in range(NCORES):
        kc, qc = build_kq_const(sl[h])
        in_maps.append({"hT_all": hT_all, "wB": wB_for_head(w_in_l, h), "cb": cbn, "cfB": build_cfB(sl[h]),
                        "kconst": kc, "qconst": qc})
    res = run_bass_kernel_spmd(nc, in_maps, core_ids=list(range(NCORES)))
    return np.stack([r["o_loc"] for r in res.results], axis=0)


class Job:
    __slots__ = ("load", "compute", "w", "rw")

    def __init__(self, load, compute):
        self.load = load
        self.compute = compute
        self.w = None
        self.rw = None


def run_jobs(jobs, look=2):
    n = len(jobs)
    for i in range(n + look):
        if i < n and jobs[i].load is not None:
            jobs[i].load(jobs[i])
        if i - look >= 0:
            jobs[i - look].compute(jobs[i - look])


class TokState:
    pass


def tok_alloc(cx):
    T = TokState()
    T.xT = cx.sb([128, 8, TSH], F32, "xT")
    T.r_x = [[Res() for _ in range(8)] for _ in range(NSUB)]
    T.ones = cx.sb([128, 128], BF16, "ones")
    T.r_ones = Res()
    T.epsc = cx.sb([128, 1], F32, "epsc")
    T.sqb = cx.sb([128, 8, TT], BF16, "sqb")
    T.r_sq = Res()
    T.rstd = cx.sb([128, TT], F32, "rstd")
    T.r_rstd = Res()
    T.hout = [cx.sb([128, 8, TT], BF16, "hout") for _ in range(2)]
    T.r_hout = mkres(2)
    T.banks = cx.get_banks()
    T.r_banks = mkres(8, True)
    T.bank_i = 0
    T.gv = cx.sb([128, 8 * (2 * DEPTH + 1)], F32, "gains")
    T.r_gv = Res()
    return T


def next_bank(T):
    i = T.bank_i % 8
    T.bank_i += 1
    return T.banks[i], T.r_banks[i]


def emit_norm(cx, T, s, gidx, dst, r_dst, out_dt_bf16=True):
    S_ = cx.S
    tsl = slice(s * TT, (s + 1) * TT)
    pt, rp = next_bank(T)
    for k in range(8):
        S_.op("pool", lambda e, k=k: e.tensor_tensor(out=T.sqb[:, k, :], in0=T.xT[:, k, tsl], in1=T.xT[:, k, tsl], op=ALU.mult),
              [T.r_x[s][k]], [T.r_sq])
    for k in range(8):
        mm(S_, pt[:, :], T.ones[:, :], T.sqb[:, k, :], k == 0, k == 7, [T.r_ones, T.r_sq], [rp])
    S_.op("act", lambda e: e.activation(out=T.rstd[:], in_=pt[:, :], func=AF.Sqrt, bias=T.epsc[:, 0:1], scale=1.0 / D),
          [rp, T.r_ones], [T.r_rstd])
    S_.op("dve", lambda e: e.reciprocal(out=T.rstd[:], in_=T.rstd[:]), [T.r_rstd], [T.r_rstd])
    for k in range(8):
        S_.op("dve", lambda e, k=k: e.scalar_tensor_tensor(out=dst[:, k, :], in0=T.xT[:, k, tsl],
                                                           scalar=T.gv[:, gidx * 8 + k:gidx * 8 + k + 1], in1=T.rstd[:],
                                                           op0=ALU.mult, op1=ALU.mult),
              [T.r_x[s][k], T.r_gv, T.r_rstd], [r_dst])


def tok_load_x(cx, T, xT_d, gains_d, cb_d, r_src=None):
    S_ = cx.S
    xv = xT_d.rearrange("(k p) t -> p k t", p=128)
    for k in range(8):
        S_.dma("sp", T.xT[:, k, :], xv[:, k, :], [r_src] if r_src else [], [T.r_x[s][k] for s in range(NSUB)])
    S_.dma("sp", T.gv[:], gains_d, [], [T.r_gv])
    S_.dma("sp", T.ones[:], cb_d[:, CB_ONES:CB_ONES + 128], [], [T.r_ones])
    S_.op("pool", lambda e: e.memset(T.epsc[:], EPS), [], [T.r_ones])


def phase_A(cx, T, gidx, hT_out_d, r_hT_loc=None):
    S_ = cx.S
    hv = hT_out_d.rearrange("(k p) t -> p k t", p=128)
    for s in range(NSUB):
        ho = T.hout[s % 2]
        rh = T.r_hout[s % 2]
        emit_norm(cx, T, s, gidx, ho, rh)
        S_.dma("sp", hv[:, :, s * TT:(s + 1) * TT], ho[:], [rh], [r_hT_loc[s]] if r_hT_loc else [])


def phase_C(cx, T, W, l, hT_loc_d, hh_d, o_sh_d, final, out_d, CS=None, dyn=None):
    S_ = cx.S
    if CS is None:
        CS = TokState()
        CS.hT = cx.sb([128, 8, TT], BF16, "hT")
        CS.hh = cx.sb([128, 8, 2], BF16, "hh")
        CS.ost = cx.sb([128, 8, TT], BF16, "ost")
        CS.r_ost = Res()
        CS.ocT = cx.sb([128, 4, TT], BF16, "ocT")
        CS.mT = cx.sb([128, 8, TT], BF16, "mT")
        CS.aT = cx.sb([128, NFC, TT], BF16, "aT")
        CS.wr = [cx.sb([128, NFC * 128], BF16, "wr") for _ in range(4)]
        CS.r_wr = mkres(4)
        CS.wr_i = 0
        CS.gs = [cx.sb([128, TT], F32, "gs") for _ in range(3)]
        CS.r_gs = mkres(3)
        CS.t = [cx.sb([128, TT], F32, "tt") for _ in range(3)]
        CS.r_t = mkres(3)
        CS.xc = cx.sb([128, TT], F32, "xc")
        CS.u = cx.sb([128, TT + 2], F32, "u")
        CS.y = cx.sb([128, TT], F32, "y")
        CS.hu = cx.sb([128, 4, 2], F32, "hu")
        CS.xh = cx.sb([128, 2], F32, "xh")
        CS.sil = cx.sb([128, TT], F32, "sil")
        CS.fout = cx.sb([128, 8, TT], F32, "fout")
        CS.bg = cx.sb([128, 24 * DEPTH], F32, "bg")
        CS.cw = cx.sb([128, 12 * DEPTH], F32, "cw")
        CS.r_hT, CS.r_hh, CS.r_ocT, CS.r_mT = Res(), Res(), Res(), Res()
        CS.r_aT = mkres(NFC)
        CS.r_xc, CS.r_u, CS.r_y, CS.r_hu, CS.r_xh, CS.r_sil, CS.r_fout, CS.r_bg, CS.r_cw = (Res() for _ in range(9))
        S_.dma("sp", CS.bg[:], W["bg"], [], [CS.r_bg])
        S_.dma("sp", CS.cw[:], W["cw"], [], [CS.r_cw])
    C = CS
    w_in = W["w_in"][l]
    hv = hT_loc_d.rearrange("(k p) t -> p k t", p=128)
    if dyn is None:
        S_.dma("sp", C.hh[:], hh_d, [], [C.r_hh])
        r_hl = [Res() for _ in range(NSUB)]
    else:
        r_hl = dyn["r_hT_loc"]
        hhraw = cx.sb([128, 8, 2], BF16, "hhraw")
        hm = cx.sb([128, 1], F32, "hm")
        r_hhraw, r_hm = Res(), Res()
        hT_all3 = dyn["hT_all3"]
        S_.dma("sp", hhraw[:], lambda d: hT_all3[:, bass.ds(d["prev"], 1), :, TSH - 2:TSH].rearrange("k a p t -> p (k a) t"),
               [dyn["r_hT_all"]], [r_hhraw])
        S_.dma("sp", hm[:], dyn["hmask"], [], [r_hm])
        S_.op("dve", lambda e: e.tensor_scalar(out=C.hh[:], in0=hhraw[:], scalar1=hm[:, 0:1], scalar2=None, op0=ALU.mult),
              [r_hhraw, r_hm], [C.r_hh])

    def slab(src, K):
        def load(job):
            i = C.wr_i % 4
            C.wr_i += 1
            view = C.wr[i][:, 0:K * 128].rearrange("p (k n) -> p k n", n=128)
            S_.dma("pool", view, src.rearrange("(k p) n -> p k n", p=128), [], [C.r_wr[i]])
            job.w = view
            job.rw = C.r_wr[i]
        return load

    def slab3(srcs):
        def load(job):
            i = C.wr_i % 4
            C.wr_i += 1
            view = C.wr[i][:, 0:12 * 128].rearrange("p (k n) -> p k n", n=128)
            S_.dma("pool", view[0:64, 0:8, :], srcs[0].rearrange("(h d) n -> d h n", d=64), [], [C.r_wr[i]])
            S_.dma("pool", view[64:128, 0:8, :], srcs[1].rearrange("(h d) n -> d h n", d=64), [], [C.r_wr[i]])
            S_.dma("pool", view[:, 8:12, :], srcs[2].rearrange("(k p) n -> p k n", p=128), [], [C.r_wr[i]])
            job.w = view
            job.rw = C.r_wr[i]
        return load

    def group(pt, rp, wv, koff, K, rhs_of, rreads, ncol=TT):
        for k in range(K):
            mm(S_, pt[:, 0:ncol], wv[:, koff + k, :], rhs_of(k), k == 0, k == K - 1, rreads, [rp])

    jobs = []
    for s in range(NSUB):
        t0 = s * TT
        tsl = slice(t0, t0 + TT)

        def load_acts(job, s=s, t0=t0):
            S_.dma("sp", C.hT[:], hv[:, :, t0:t0 + TT], [r_hl[s]], [C.r_hT])
            if dyn is None:
                S_.dma("sp", C.ost[:], o_sh_d[:, :, t0:t0 + TT].rearrange("h r t -> r h t"), [], [C.r_ost])
            else:
                o3 = dyn["o_all3"]
                S_.dma("sp", C.ost[:], lambda d: o3[bass.ds(d["pid"], 1), :, :, t0:t0 + TT].rearrange("a h r t -> r (a h) t"),
                       dyn["r_o_all"], [C.r_ost])
        jobs.append(Job(load_acts, lambda job: None))

        for i in range(4):
            def c_xc(job, s=s, i=i):
                pt, rp = next_bank(T)
                group(pt, rp, job.w, 0, 8, lambda k: C.hT[:, k, :], [job.rw, C.r_hT])
                S_.op("act", lambda e: e.copy(out=C.xc[:], in_=pt[:, :]), [rp], [C.r_xc])
                if s == 0:
                    p2, rp2 = next_bank(T)
                    group(p2, rp2, job.w, 0, 8, lambda k: C.hh[:, k, :], [job.rw, C.r_hh], ncol=2)
                    S_.op("act", lambda e: e.copy(out=C.xh[:], in_=p2[:, 0:2]), [rp2], [C.r_xh])
            jobs.append(Job(slab(w_in[:, 3072 + i * 128:3072 + (i + 1) * 128], 8), c_xc))

            def c_cc(job, s=s, i=i):
                pt, rp = next_bank(T)
                group(pt, rp, job.w, 0, 8, lambda k: C.hT[:, k, :], [job.rw, C.r_hT])
                if s == 0:
                    p2, rp2 = next_bank(T)
                    group(p2, rp2, job.w, 0, 8, lambda k: C.hh[:, k, :], [job.rw, C.r_hh], ncol=2)
                    S_.op("dve", lambda e: e.tensor_tensor(out=C.hu[:, i, :], in0=p2[:, 0:2], in1=C.xh[:], op=ALU.mult),
                          [rp2, C.r_xh], [C.r_hu])
                S_.op("dve", lambda e: e.tensor_copy(out=C.u[:, 0:2], in_=C.hu[:, i, :]), [C.r_hu], [C.r_u])
                S_.op("dve", lambda e: e.tensor_tensor(out=C.u[:, 2:TT + 2], in0=pt[:, :], in1=C.xc[:], op=ALU.mult),
                      [rp, C.r_xc], [C.r_u])
                S_.op("dve", lambda e: e.tensor_copy(out=C.hu[:, i, :], in_=C.u[:, TT:TT + 2]), [C.r_u], [C.r_hu])
                cw0 = l * 12 + i * 3
                S_.op("dve", lambda e: e.tensor_scalar(out=C.y[:], in0=C.u[:, 0:TT], scalar1=C.cw[:, cw0:cw0 + 1], scalar2=None,
                                                       op0=ALU.mult), [C.r_u, C.r_cw], [C.r_y])
                S_.op("dve", lambda e: e.scalar_tensor_tensor(out=C.y[:], in0=C.u[:, 1:TT + 1], scalar=C.cw[:, cw0 + 1:cw0 + 2],
                                                              in1=C.y[:], op0=ALU.mult, op1=ALU.add), [C.r_u, C.r_cw, C.r_y], [C.r_y])
                S_.op("dve", lambda e: e.scalar_tensor_tensor(out=C.y[:], in0=C.u[:, 2:TT + 2], scalar=C.cw[:, cw0 + 2:cw0 + 3],
                                                              in1=C.y[:], op0=ALU.mult, op1=ALU.add), [C.r_u, C.r_cw, C.r_y], [C.r_y])
            jobs.append(Job(slab(w_in[:, 4096 + i * 128:4096 + (i + 1) * 128], 8), c_cc))

            def c_bc(job, i=i):
                pt, rp = next_bank(T)
                group(pt, rp, job.w, 0, 8, lambda k: C.hT[:, k, :], [job.rw, C.r_hT])
                S_.op("dve", lambda e: e.tensor_tensor(out=C.ocT[:, i, :], in0=pt[:, :], in1=C.y[:], op=ALU.mult),
                      [rp, C.r_y], [C.r_ocT])
            jobs.append(Job(slab(w_in[:, 3584 + i * 128:3584 + (i + 1) * 128], 8), c_bc))

        for oc in range(8):
            for br in range(3):
                def c_gate(job, oc=oc, br=br):
                    pt, rp = next_bank(T)
                    group(pt, rp, job.w, 0, 8, lambda k: C.hT[:, k, :], [job.rw, C.r_hT])
                    bcol = l * 24 + br * 8 + oc
                    S_.op("act", lambda e: e.activation(out=C.gs[br][:], in_=pt[:, :], func=AF.Sigmoid,
                                                        bias=C.bg[:, bcol:bcol + 1], scale=1.0),
                          [rp, C.r_bg], [C.r_gs[br]])
                c0 = 4608 + br * 1024 + oc * 128
                jobs.append(Job(slab(w_in[:, c0:c0 + 128], 8), c_gate))

            def c_proj(job, oc=oc):
                for br in range(3):
                    pt, rp = next_bank(T)
                    if br < 2:
                        ps_ = slice(64 * br, 64 * br + 64)
                        for h in range(8):
                            mm(S_, pt[:, :], job.w[ps_, h, :], C.ost[ps_, h, :], h == 0, h == 7, [job.rw, C.r_ost], [rp])
                    else:
                        for k in range(4):
                            mm(S_, pt[:, :], job.w[:, 8 + k, :], C.ocT[:, k, :], k == 0, k == 3, [job.rw, C.r_ocT], [rp])
                    S_.op("dve", lambda e, br=br, pt=pt: e.tensor_tensor(out=C.t[br][:], in0=pt[:, :], in1=C.gs[br][:], op=ALU.mult),
                          [rp, C.r_gs[br]], [C.r_t[br]])
                S_.op("pool", lambda e: e.tensor_tensor(out=C.t[0][:], in0=C.t[0][:], in1=C.t[1][:], op=ALU.add),
                      [C.r_t[0], C.r_t[1]], [C.r_t[0]])
                S_.op("pool", lambda e: e.tensor_tensor(out=C.mT[:, oc, :], in0=C.t[0][:], in1=C.t[2][:], op=ALU.add),
                      [C.r_t[0], C.r_t[2]], [C.r_mT])
            jobs.append(Job(slab3([W["wpa"][l][:, oc * 128:(oc + 1) * 128], W["wps"][l][:, oc * 128:(oc + 1) * 128],
                                   W["wpc"][l][:, oc * 128:(oc + 1) * 128]]), c_proj))

        for oc in range(8):
            def c_wo(job, s=s, oc=oc, tsl=tsl):
                pt, rp = next_bank(T)
                group(pt, rp, job.w, 0, 8, lambda k: C.mT[:, k, :], [job.rw, C.r_mT])
                S_.op("dve", lambda e: e.tensor_tensor(out=T.xT[:, oc, tsl], in0=pt[:, :], in1=T.xT[:, oc, tsl], op=ALU.add),
                      [rp, T.r_x[s][oc]], [T.r_x[s][oc]])
            jobs.append(Job(slab(W["w_out"][l][:, oc * 128:(oc + 1) * 128], 8), c_wo))

        def c_norm2(job, s=s):
            emit_norm(cx, T, s, 2 * l + 1, C.hT, C.r_hT)
        jobs.append(Job(None, c_norm2))

        for fc in range(NFC):
            def c_fg(job, fc=fc):
                pt, rp = next_bank(T)
                group(pt, rp, job.w, 0, 8, lambda k: C.hT[:, k, :], [job.rw, C.r_hT])
                S_.op("act", lambda e: e.activation(out=C.sil[:], in_=pt[:, :], func=AF.Silu), [rp], [C.r_sil])
            jobs.append(Job(slab(W["wg"][l][:, fc * 128:(fc + 1) * 128], 8), c_fg))

            def c_fu(job, fc=fc):
                pt, rp = next_bank(T)
                group(pt, rp, job.w, 0, 8, lambda k: C.hT[:, k, :], [job.rw, C.r_hT])
                S_.op("dve", lambda e: e.tensor_tensor(out=C.aT[:, fc, :], in0=pt[:, :], in1=C.sil[:], op=ALU.mult),
                      [rp, C.r_sil], [C.r_aT[fc]])
            jobs.append(Job(slab(W["wu"][l][:, fc * 128:(fc + 1) * 128], 8), c_fu))

        for oc in range(8):
            def c_fd(job, s=s, oc=oc, tsl=tsl):
                pt, rp = next_bank(T)
                for k in range(NFC):
                    mm(S_, pt[:, :], job.w[:, k, :], C.aT[:, k, :], k == 0, k == NFC - 1, [job.rw, C.r_aT[k]], [rp])
                S_.op("dve", lambda e: e.tensor_tensor(out=T.xT[:, oc, tsl], in0=pt[:, :], in1=T.xT[:, oc, tsl], op=ALU.add),
                      [rp, T.r_x[s][oc]], [T.r_x[s][oc]])
            jobs.append(Job(slab(W["wd"][l][:, oc * 128:(oc + 1) * 128], NFC), c_fd))

        def c_out(job, s=s, t0=t0):
            if final:
                emit_norm(cx, T, s, 2 * DEPTH, C.fout, C.r_fout)
                S_.dma("sp", out_d.rearrange("(k p) t -> p k t", p=128)[:, :, t0:t0 + TT], C.fout[:], [C.r_fout], [])
            else:
                ho = T.hout[s % 2]
                rh = T.r_hout[s % 2]
                emit_norm(cx, T, s, 2 * (l + 1), ho, rh)
                S_.dma("sp", out_d.rearrange("(k p) t -> p k t", p=128)[:, :, t0:t0 + TT], ho[:], [rh], [r_hl[s]])
        jobs.append(Job(None, c_out))
    run_jobs(jobs, look=2)
    return CS


NG = 2 * DEPTH + 1


def declare_weights(nc):
    W = {}
    W["w_in"] = nc.dram_tensor("w_in", [DEPTH, D, INW], F32, kind="ExternalInput").ap()
    W["wpa"] = nc.dram_tensor("wpa", [DEPTH, 512, D], F32, kind="ExternalInput").ap()
    W["wps"] = nc.dram_tensor("wps", [DEPTH, 512, D], F32, kind="ExternalInput").ap()
    W["wpc"] = nc.dram_tensor("wpc", [DEPTH, 512, D], F32, kind="ExternalInput").ap()
    W["w_out"] = nc.dram_tensor("w_out", [DEPTH, D, D], F32, kind="ExternalInput").ap()
    W["wg"] = nc.dram_tensor("wg", [DEPTH, D, DFF], F32, kind="ExternalInput").ap()
    W["wu"] = nc.dram_tensor("wu", [DEPTH, D, DFF], F32, kind="ExternalInput").ap()
    W["wd"] = nc.dram_tensor("wd", [DEPTH, DFF, D], F32, kind="ExternalInput").ap()
    W["bg"] = nc.dram_tensor("bg", [128, 24 * DEPTH], F32, kind="ExternalInput").ap()
    W["cw"] = nc.dram_tensor("cw", [128, 12 * DEPTH], F32, kind="ExternalInput").ap()
    return W


def host_weights(inp):
    f = lambda a: np.ascontiguousarray(np.asarray(a, dtype=np.float32))
    bg = f(inp["b_gate"]).reshape(DEPTH, 24, 128).transpose(2, 0, 1).reshape(128, DEPTH * 24)
    cw = f(inp["conv_w"]).reshape(DEPTH, 3, 4, 128).transpose(3, 0, 2, 1).reshape(128, DEPTH * 12)
    return {"w_in": f(inp["w_in"]), "wpa": f(inp["w_proj_moba"]), "wps": f(inp["w_proj_sb"]), "wpc": f(inp["w_proj_conv"]),
            "w_out": f(inp["w_out"]), "wg": f(inp["w_ffn_gate"]), "wu": f(inp["w_ffn_up"]), "wd": f(inp["w_ffn_down"]),
            "bg": f(bg), "cw": f(cw)}


def host_gains(inp):
    G = [inp["norm_mix_g"][0], inp["norm_ffn_g"][0], inp["norm_mix_g"][1], inp["norm_ffn_g"][1], inp["norm_final_g"]]
    G = np.stack([np.asarray(g, np.float32) for g in G], 0)
    return np.ascontiguousarray(G.reshape(NG, 8, 128).transpose(2, 0, 1).reshape(128, NG * 8))


def build_A():
    nc = bass.Bass("TRN2", target_bir_lowering=False)
    xT_d = nc.dram_tensor("xT", [D, TSH], F32, kind="ExternalInput").ap()
    gains_d = nc.dram_tensor("gains", [128, NG * 8], F32, kind="ExternalInput").ap()
    cb_d = nc.dram_tensor("cb", [128, CB_W], BF16, kind="ExternalInput").ap()
    hT_o = nc.dram_tensor("hT_o", [D, TSH], BF16, kind="ExternalOutput").ap()
    with ExitStack() as stack:
        cx = Ctx(nc, stack)
        T = tok_alloc(cx)
        tok_load_x(cx, T, xT_d, gains_d, cb_d)
        phase_A(cx, T, 0, hT_o)
        cx.S.finish("sp")
        cx.S.emit(nc, stack)
    return nc


def build_C(l, final):
    nc = bass.Bass("TRN2", target_bir_lowering=False)
    xT_d = nc.dram_tensor("xT", [D, TSH], F32, kind="ExternalInput").ap()
    gains_d = nc.dram_tensor("gains", [128, NG * 8], F32, kind="ExternalInput").ap()
    cb_d = nc.dram_tensor("cb", [128, CB_W], BF16, kind="ExternalInput").ap()
    hT_d = nc.dram_tensor("hT_loc", [D, TSH], BF16, kind="ExternalInput").ap()
    hh_d = nc.dram_tensor("hh", [128, 8, 2], BF16, kind="ExternalInput").ap()
    o_sh = nc.dram_tensor("o_sh", [NCORES, 128, TSH], BF16, kind="ExternalInput").ap()
    W = declare_weights(nc)
    if final:
        out_d = nc.dram_tensor("outT", [D, TSH], F32, kind="ExternalOutput").ap()
    else:
        out_d = nc.dram_tensor("hT_o", [D, TSH], BF16, kind="ExternalOutput").ap()
        x2_d = nc.dram_tensor("x2T", [D, TSH], F32, kind="ExternalOutput").ap()
    with ExitStack() as stack:
        cx = Ctx(nc, stack)
        T = tok_alloc(cx)
        tok_load_x(cx, T, xT_d, gains_d, cb_d)
        phase_C(cx, T, W, l, hT_d, hh_d, o_sh, final, out_d)
        if not final:
            xv = x2_d.rearrange("(k p) t -> p k t", p=128)
            for k in range(8):
                cx.S.dma("sp", xv[:, k, :], T.xT[:, k, :], [T.r_x[s][k] for s in range(NSUB)], [])
        cx.S.finish("sp")
        cx.S.emit(nc, stack)
    return nc


def halo_from_hT(hT_sh):
    out = []
    for r in range(NCORES):
        if r % (NCORES // B) == 0:
            hh = np.zeros((D, 2), ml_dtypes.bfloat16)
        else:
            hh = hT_sh[r - 1][:, TSH - 2:TSH]
        out.append(np.ascontiguousarray(hh.reshape(8, 128, 2).transpose(1, 0, 2)))
    return out


def kernel_unfused(**inp):
    x = np.asarray(inp["x"], np.float32)
    cores = list(range(NCORES))
    xT_sh = [np.ascontiguousarray(x.reshape(NTOK, D)[r * TSH:(r + 1) * TSH].T) for r in cores]
    gains = host_gains(inp)
    cbn = build_cb()
    Wh = host_weights(inp)
    w_in = np.asarray(inp["w_in"], np.float32)
    res = run_bass_kernel_spmd(build_A(), [{"xT": xT_sh[r], "gains": gains, "cb": cbn} for r in cores], core_ids=cores)
    hT_sh = [res.results[r]["hT_o"] for r in cores]
    out = None
    for l in range(DEPTH):
        hT_all = np.ascontiguousarray(np.stack(hT_sh, 0))
        o_all = run_B(hT_all, w_in[l])
        halos = halo_from_hT(hT_sh)
        final = (l == DEPTH - 1)
        in_maps = []
        for r in cores:
            m = {"xT": xT_sh[r], "gains": gains, "cb": cbn, "hT_loc": hT_sh[r], "hh": halos[r],
                 "o_sh": np.ascontiguousarray(o_all[:, :, r * TSH:(r + 1) * TSH])}
            m.update(Wh)
            in_maps.append(m)
        res = run_bass_kernel_spmd(build_C(l, final), in_maps, core_ids=cores)
        if final:
            out = np.concatenate([res.results[r]["outT"].T for r in cores], axis=0)
        else:
            hT_sh = [res.results[r]["hT_o"] for r in cores]
            xT_sh = [res.results[r]["x2T"] for r in cores]
    return np.ascontiguousarray(out.reshape(B, S, D).astype(np.float32))


def build_fused():
    nc = bass.Bass("TRN2", target_bir_lowering=False)
    xT_d = nc.dram_tensor("xT", [D, TSH], F32, kind="ExternalInput").ap()
    gains_d = nc.dram_tensor("gains", [128, NG * 8], F32, kind="ExternalInput").ap()
    cb_d = nc.dram_tensor("cb", [128, CB_W], BF16, kind="ExternalInput").ap()
    cfB_d = nc.dram_tensor("cfB", [128, 128], F32, kind="ExternalInput").ap()
    kconst_d = nc.dram_tensor("kconst", [35, S], BF16, kind="ExternalInput").ap()
    qconst_d = nc.dram_tensor("qconst", [2, S], BF16, kind="ExternalInput").ap()
    hmask_d = nc.dram_tensor("hmask", [128, 1], F32, kind="ExternalInput").ap()
    wB_d = [nc.dram_tensor("wB%d" % l, [D, 384], F32, kind="ExternalInput").ap() for l in range(DEPTH)]
    W = declare_weights(nc)
    outT = nc.dram_tensor("outT", [D, TSH], F32, kind="ExternalOutput").ap()
    hT_loc = nc.dram_tensor("hT_loc_i", [D, TSH], BF16).ap()
    hT_all = nc.dram_tensor("hT_all_i", [NCORES * D, TSH], BF16).ap()
    o_loc = nc.dram_tensor("o_loc_i", [128, NTOK], BF16).ap()
    o_all = nc.dram_tensor("o_all_i", [NCORES * NCORES * 128, TSH], BF16).ap()
    x2_d = nc.dram_tensor("x2_i", [D, TSH], F32).ap()
    global HT_CHUNKED
    HT_CHUNKED = True
    hT_all3 = hT_all.rearrange("(k r p) t -> k r p t", k=8, r=NCORES)
    o_all3 = o_all.rearrange("(c r p) t -> c r p t", c=NCORES, r=NCORES)
    o_loc2 = o_loc
    o_loc = nc.dram_tensor("o_locc_i", [NCORES * 128, TSH], BF16).ap()
    o_loc3 = o_loc.rearrange("(c p) t -> c p t", c=NCORES)

    def gather_h():
        for k in range(8):
            S_.coll("AllGather", [hT_loc[k * 128:(k + 1) * 128, :]], [hT_all[k * 1024:(k + 1) * 1024, :]], r_hT_loc, [r_hT_all])

    def gather_o_shard(c):
        S_.coll("AllGather", [o_loc[c * 128:(c + 1) * 128, :]], [o_all[c * 1024:(c + 1) * 1024, :]], [r_o_loc[c]], [r_o_all[c]])
    with ExitStack() as stack:
        cx = Ctx(nc, stack)
        S_ = cx.S
        cx.get_banks()
        r_hT_loc = mkres(NSUB)
        r_hT_all, r_x2 = Res(), Res()
        r_o_loc = mkres(NCORES)
        r_o_all = mkres(NCORES)
        m0 = cx.mark()
        T = tok_alloc(cx)
        tok_load_x(cx, T, xT_d, gains_d, cb_d)
        phase_A(cx, T, 0, hT_loc, r_hT_loc)
        gather_h()
        S_.barrier()
        cx.release(m0)
        for l in range(DEPTH):
            final = (l == DEPTH - 1)
            phase_B(cx, hT_all3, wB_d[l], cb_d, cfB_d, kconst_d, qconst_d, o_loc3, r_hT_all, r_o_loc, do_finish=False,
                    shard_done=gather_o_shard)
            S_.barrier()
            cx.release(m0)
            T = tok_alloc(cx)
            tok_load_x(cx, T, xT_d if l == 0 else x2_d, gains_d, cb_d, None if l == 0 else r_x2)
            dyn = {"r_hT_loc": r_hT_loc, "hT_all3": hT_all3, "r_hT_all": r_hT_all, "hmask": hmask_d, "o_all3": o_all3, "r_o_all": r_o_all}
            phase_C(cx, T, W, l, hT_loc, None, None, final, outT if final else hT_loc, dyn=dyn)
            if not final:
                xv = x2_d.rearrange("(k p) t -> p k t", p=128)
                for k in range(8):
                    S_.dma("sp", xv[:, k, :], T.xT[:, k, :], [T.r_x[s][k] for s in range(NSUB)], [r_x2])
                gather_h()
            S_.barrier()
            cx.release(m0)
        S_.finish("sp")
        print("SBUF peak", cx.peak)
        S_.emit(nc, stack)
    return nc


def kernel_fused(**inp):
    x = np.asarray(inp["x"], np.float32)
    cores = list(range(NCORES))
    gains = host_gains(inp)
    cbn = build_cb()
    Wh = host_weights(inp)
    w_in = np.asarray(inp["w_in"], np.float32)
    sl = slopes()
    in_maps = []
    for r in cores:
        kc, qc = build_kq_const(sl[r])
        m = {"xT": np.ascontiguousarray(x.reshape(NTOK, D)[r * TSH:(r + 1) * TSH].T), "gains": gains, "cb": cbn,
             "cfB": build_cfB(sl[r]), "kconst": kc, "qconst": qc,
             "hmask": np.full((128, 1), 0.0 if r % (NCORES // B) == 0 else 1.0, np.float32)}
        for l in range(DEPTH):
            m["wB%d" % l] = wB_for_head(w_in[l], r)
        m.update(Wh)
        in_maps.append(m)
    res = run_bass_kernel_spmd(build_fused(), in_maps, core_ids=cores)
    out = np.concatenate([res.results[r]["outT"].T for r in cores], axis=0)
    return np.ascontiguousarray(out.reshape(B, S, D).astype(np.float32))


def kernel(**inp):
    return kernel_fused(**inp)
```
